# Optimizing a Trainium2 kernel written in Bass

```python
import jax, jax.numpy as jnp
from jax import lax
import numpy as np

D_MODEL = 1024
BATCH = 32
SEQ = 2048
DEPTH = 1

N_HEADS = 16
N_KV_HEADS = 4
HEAD_DIM = 64
D_ATTN = N_HEADS * HEAD_DIM
D_KV = N_KV_HEADS * HEAD_DIM
WINDOW = 128
BLOCK = 128
KSPAN = BLOCK + 2 * WINDOW
NEG_INF = -1e30
D_CONV = D_MODEL
CONV_WIDTH = 3
N_EXPERTS = 16
CAPACITY_FACTOR = 2
D_FF_EXPERT = 2048
DEEPNORM_ALPHA = (2.0 * DEPTH) ** 0.25
DEEPNORM_BETA = (8.0 * DEPTH) ** -0.25
LN_EPS = 1e-5
IN_SPLITS = (D_ATTN, D_KV, D_KV, D_CONV, D_CONV, D_CONV, D_MODEL, D_MODEL)
D_IN = sum(IN_SPLITS)
SPLIT_IDX = [int(c) for c in np.cumsum(IN_SPLITS)[:-1]]

kernel_name = "hybrid_conv_swa_ec_moe_deepnorm"


def layer_norm(x, g, b):
    xf = x.astype(jnp.float32)
    mu = xf.mean(-1, keepdims=True)
    var = jnp.square(xf - mu).mean(-1, keepdims=True)
    y = (xf - mu) * lax.rsqrt(var + LN_EPS) * g.astype(jnp.float32) + b.astype(jnp.float32)
    return y.astype(x.dtype)


def alibi_slopes(n_heads):
    return (2.0 ** (-8.0 * np.arange(1, n_heads + 1) / n_heads)).astype(np.float32)


def windowed_gqa(q, k, v, sink):
    B, S, _ = q.shape
    nb = S // BLOCK
    rep = N_HEADS // N_KV_HEADS
    qb = q.reshape(B, nb, BLOCK, N_KV_HEADS, rep, HEAD_DIM).astype(jnp.float32) * (HEAD_DIM ** -0.5)
    qb = jnp.moveaxis(qb, 1, 0)
    pad = ((0, 0), (WINDOW, WINDOW), (0, 0), (0, 0))
    kp = jnp.pad(k.reshape(B, S, N_KV_HEADS, HEAD_DIM).astype(jnp.float32), pad)
    vp = jnp.pad(v.reshape(B, S, N_KV_HEADS, HEAD_DIM).astype(jnp.float32), pad)
    slopes = jnp.asarray(alibi_slopes(N_HEADS)).reshape(N_KV_HEADS, rep)
    i = jnp.arange(BLOCK)[:, None]
    j = jnp.arange(KSPAN)[None, :]
    dist = jnp.abs(i - j + WINDOW)
    bias = -slopes[:, :, None, None] * dist.astype(jnp.float32)
    sink = sink.astype(jnp.float32).reshape(N_KV_HEADS, rep)[None, :, :, None]

    def one_block(args):
        q_blk, n = args
        start = n * BLOCK
        k_blk = lax.dynamic_slice_in_dim(kp, start, KSPAN, axis=1)
        v_blk = lax.dynamic_slice_in_dim(vp, start, KSPAN, axis=1)
        kpos = start - WINDOW + j
        valid = (dist <= WINDOW) & (kpos >= 0) & (kpos < S)
        s = jnp.einsum('bqgrd,bkgd->bgrqk', q_blk, k_blk) + bias
        s = jnp.where(valid, s, NEG_INF)
        m = jnp.maximum(s.max(-1), sink)
        p = jnp.exp(s - m[..., None])
        denom = p.sum(-1) + jnp.exp(sink - m)
        return jnp.einsum('bgrqk,bkgd->bqgrd', p / denom[..., None], v_blk)

    o = lax.map(one_block, (qb, jnp.arange(nb)))
    return jnp.moveaxis(o, 0, 1).reshape(B, S, D_ATTN).astype(q.dtype)


def short_conv(b_gate, c_gate, xc, conv_w):
    u = c_gate * xc
    S = u.shape[1]
    up = jnp.pad(u, ((0, 0), (CONV_WIDTH // 2, CONV_WIDTH // 2), (0, 0)))
    y = conv_w[0] * up[:, 0:S]
    for w in range(1, CONV_WIDTH):
        y = y + conv_w[w] * up[:, w:w + S]
    return b_gate * y


def hybrid_mixer(x, w_in, conv_w, attn_sink, w_attn_o, w_conv_o, w_out):
    proj = x @ w_in
    q, k, v, cb, cc, cx, ga, gc = jnp.split(proj, SPLIT_IDX, axis=-1)
    y_attn = windowed_gqa(q, k, v, attn_sink) @ w_attn_o
    y_conv = short_conv(cb, cc, cx, conv_w) @ w_conv_o
    merged = jax.nn.sigmoid(ga) * y_attn + jax.nn.sigmoid(gc) * y_conv
    return merged @ w_out


def expert_choice_moe(x, w_router, w_gate, w_up, w_down):
    B, S, _ = x.shape
    cap = CAPACITY_FACTOR * S // N_EXPERTS
    logits = jnp.einsum('bsd,de->bse', x, w_router).astype(jnp.float32)
    aff = jax.nn.softmax(logits, axis=-1)
    top_val, top_idx = lax.top_k(jnp.swapaxes(aff, 1, 2), cap)
    b_idx = jnp.arange(B)[:, None, None]
    xe = x[b_idx, top_idx]
    h = jax.nn.silu(jnp.einsum('becd,edf->becf', xe, w_gate)) * jnp.einsum('becd,edf->becf', xe, w_up)
    ye = jnp.einsum('becf,efd->becd', h, w_down) * top_val[..., None].astype(x.dtype)
    return jnp.zeros_like(x).at[b_idx, top_idx].add(ye)


def setup_inputs(seed: int = 0) -> dict:
    key = jax.random.key(seed)
    ks = jax.random.split(key, 18)
    f32 = jnp.float32
    nrm = lambda k, s: jax.random.normal(k, s, f32)
    L = DEPTH
    col_scale = np.ones((D_IN,), np.float32)
    col_scale[D_ATTN + D_KV:D_ATTN + 2 * D_KV] = DEEPNORM_BETA
    w_in = nrm(ks[3], (L, D_MODEL, D_IN)) * (D_MODEL ** -0.5) * jnp.asarray(col_scale)
    return {
        "x": nrm(ks[0], (BATCH, SEQ, D_MODEL)),
        "ln0_g": 1.0 + 0.02 * nrm(ks[1], (D_MODEL,)),
        "ln0_b": 0.02 * nrm(ks[2], (D_MODEL,)),
        "w_in": w_in,
        "conv_w": nrm(ks[4], (L, CONV_WIDTH, D_CONV)) * (CONV_WIDTH ** -0.5),
        "attn_sink": 0.5 * nrm(ks[5], (L, N_HEADS)),
        "w_attn_o": nrm(ks[6], (L, D_ATTN, D_MODEL)) * (D_ATTN ** -0.5) * DEEPNORM_BETA,
        "w_conv_o": nrm(ks[7], (L, D_CONV, D_MODEL)) * (D_CONV ** -0.5) * DEEPNORM_BETA,
        "w_out": nrm(ks[8], (L, D_MODEL, D_MODEL)) * (D_MODEL ** -0.5) * DEEPNORM_BETA,
        "ln1_g": 1.0 + 0.02 * nrm(ks[9], (L, D_MODEL)),
        "ln1_b": 0.02 * nrm(ks[10], (L, D_MODEL)),
        "w_router": nrm(ks[11], (L, D_MODEL, N_EXPERTS)) * (D_MODEL ** -0.5),
        "w_gate": nrm(ks[12], (L, N_EXPERTS, D_MODEL, D_FF_EXPERT)) * (D_MODEL ** -0.5),
        "w_up": nrm(ks[13], (L, N_EXPERTS, D_MODEL, D_FF_EXPERT)) * (D_MODEL ** -0.5),
        "w_down": nrm(ks[14], (L, N_EXPERTS, D_FF_EXPERT, D_MODEL)) * (D_FF_EXPERT ** -0.5) * DEEPNORM_BETA,
        "ln2_g": 1.0 + 0.02 * nrm(ks[15], (L, D_MODEL)),
        "ln2_b": 0.02 * nrm(ks[16], (L, D_MODEL)),
    }


def reference(x, ln0_g, ln0_b, w_in, conv_w, attn_sink, w_attn_o, w_conv_o, w_out,
              ln1_g, ln1_b, w_router, w_gate, w_up, w_down, ln2_g, ln2_b):
    x = layer_norm(x, ln0_g, ln0_b)
    for l in range(DEPTH):
        h = hybrid_mixer(x, w_in[l], conv_w[l], attn_sink[l], w_attn_o[l], w_conv_o[l], w_out[l])
        x = layer_norm(DEEPNORM_ALPHA * x + h, ln1_g[l], ln1_b[l])
        f = expert_choice_moe(x, w_router[l], w_gate[l], w_up[l], w_down[l])
        x = layer_norm(DEEPNORM_ALPHA * x + f, ln2_g[l], ln2_b[l])
    return x
```

```python
import numpy as np
import concourse.bass as bass
import concourse.mybir as mybir
from concourse.bass_utils import run_bass_kernel_spmd

F32 = mybir.dt.float32
BF16 = mybir.dt.bfloat16
I32 = mybir.dt.int32
U32 = mybir.dt.uint32
AF = mybir.ActivationFunctionType
ALU = mybir.AluOpType

D = 1024
SEQ = 2048
NT16 = SEQ // 128
NEXP = 16
CAP = 256
DFF = 2048
ALPHA = 2.0 ** 0.25
EPS = 1e-5
NCORES = 8
WBIG_COLS = 9728
OQ, OKV, OCONV, OE1, OE2, OWO = 0, 1024, 1536, 4608, 6656, 8704


class Op:
    __slots__ = ("eng", "fn", "deps", "odeps", "needed", "sem", "val", "dma", "ndma", "occ", "lat", "idx", "start", "fin")

    def __init__(self, eng, fn, dma=None, ndma=0):
        self.eng = eng
        self.fn = fn
        self.deps = set()
        self.odeps = set()
        self.needed = False
        self.sem = None
        self.val = 0
        self.dma = dma
        self.ndma = ndma


class DSem:
    def __init__(self, name):
        self.name = name
        self.count = 0
        self.h = None
        self.last = None


DEFC = {"pe": 1.0, "act": 0.6, "dve": 0.65, "pool": 3.0, "sp": 0.1}


class Sched:
    ENGS = ("pe", "act", "dve", "pool", "sp")

    def __init__(self):
        self.ops = {e: [] for e in self.ENGS}
        self.all = []
        self.lastw = {}
        self.readers = {}
        self.dsems = []
        self.final_waits = []

    def dsem(self, name):
        d = DSem(name)
        self.dsems.append(d)
        return d

    def add(self, eng, fn, r=(), w=(), dma=None, ndma=1, c=None, lat=None, soft=False):
        op = Op(eng, fn, dma, ndma if dma is not None else 0)
        deps = op.deps
        for k in r:
            x = self.lastw.get(k)
            if x is not None:
                deps.add(x)
        for k in w:
            x = self.lastw.get(k)
            if x is not None:
                deps.add(x)
            rs = self.readers.get(k)
            if rs:
                deps.update(rs)
        for k in w:
            self.lastw[k] = op
            self.readers[k] = []
        for k in r:
            self.readers.setdefault(k, []).append(op)
        deps.discard(op)
        if eng == "pe":
            op.deps = deps = {d for d in deps if not (d.eng == "pe" and d.dma is None)}
        if soft:
            same = {d for d in deps if d.eng == eng and d.dma is None}
            op.odeps |= same
            op.deps = deps = deps - same
        for d in deps:
            d.needed = True
        if dma is not None:
            dma.count += 16 * ndma
            op.sem = dma
            op.val = dma.count
            if dma.last is not None and dma.last not in deps:
                op.odeps.add(dma.last)
            dma.last = op
            op.occ = c if c is not None else (0.12 if eng == "sp" else 1.0)
            op.lat = lat if lat is not None else 3.0
        else:
            op.occ = c if c is not None else DEFC[eng]
            op.lat = op.occ
        op.idx = len(self.all)
        self.all.append(op)
        return op

    def reorder(self):
        import heapq
        succ = {}
        nd = {}
        for op in self.all:
            nd[op] = len(op.deps) + len(op.odeps)
            for d in op.deps:
                succ.setdefault(d, []).append((op, 0))
            for d in op.odeps:
                succ.setdefault(d, []).append((op, 1))
        rt = {}
        avail = {e: [] for e in self.ENGS}
        future = {e: [] for e in self.ENGS}
        T = {e: 0.0 for e in self.ENGS}
        for op in self.all:
            if nd[op] == 0:
                heapq.heappush(avail[op.eng], (op.idx, op))
                rt[op] = 0.0
        order = {e: [] for e in self.ENGS}
        n = len(self.all)
        done = 0
        while done < n:
            best = None
            for e in self.ENGS:
                fu, av = future[e], avail[e]
                while fu and fu[0][0] <= T[e]:
                    _, i, o = heapq.heappop(fu)
                    heapq.heappush(av, (i, o))
                if av:
                    cand = (T[e], av[0][0], e, 0)
                elif fu:
                    cand = (fu[0][0], fu[0][1], e, 1)
                else:
                    continue
                if best is None or cand < best:
                    best = cand
            st, _, e, src = best
            if src == 0:
                _, op = heapq.heappop(avail[e])
            else:
                _, _, op = heapq.heappop(future[e])
            op.start = st
            op.fin = st + op.lat
            T[e] = st + op.occ
            order[e].append(op)
            done += 1
            for (o2, kind) in succ.get(op, ()):
                t = op.start if kind == 1 else op.fin
                if rt.get(o2, 0.0) < t:
                    rt[o2] = t
                nd[o2] -= 1
                if nd[o2] == 0:
                    heapq.heappush(future[o2.eng], (rt.get(o2, 0.0), o2.idx, o2))
        self.ops = order
        self.sim_T = max(T.values())

    def emit(self, nc, block, ctxs):
        engsem = {}
        for e in self.ENGS:
            engsem[e] = ctxs.enter_context(nc.semaphore("prog_" + e))
        for d in self.dsems:
            d.h = ctxs.enter_context(nc.semaphore("d_" + d.name))
        for e in self.ENGS:
            c = 0
            for op in self.ops[e]:
                if op.dma is None and op.needed:
                    c += 1
                    op.sem = e
                    op.val = c

        def handle(s):
            return engsem[s] if isinstance(s, str) else s.h

        def run(ename, eng):
            waited = {}
            for op in self.ops[ename]:
                need = {}
                for d in op.deps:
                    k = d.sem
                    if need.get(k, 0) < d.val:
                        need[k] = d.val
                for k, v in need.items():
                    if waited.get(k, 0) < v:
                        eng.wait_ge(handle(k), v)
                        waited[k] = v
                if op.fn is None:
                    continue
                if op.dma is not None:
                    op.fn(eng, op.dma.h)
                else:
                    ins = op.fn(eng)
                    if op.needed:
                        ins.then_inc(engsem[ename], 1)
            if ename == "sp":
                for d, v in self.final_waits:
                    eng.wait_ge(d.h, v)

        block.tensor(lambda e: run("pe", e))
        block.scalar(lambda e: run("act", e))
        block.vector(lambda e: run("dve", e))
        block.gpsimd(lambda e: run("pool", e))
        block.sync(lambda e: run("sp", e))


def bk(name, lo, hi, bs):
    return [(name, b) for b in range(lo // bs, (hi - 1) // bs + 1)]


CFG = {"lnA": "fused", "lnF": "fused", "nzF": 4, "lnFin": "act", "nfs": 5, "soft_topk": False}


def build(NSEQ, debug=False, cfg=None):
    cfg = dict(CFG, **(cfg or {}))
    from contextlib import ExitStack

    NT = NSEQ * SEQ
    nc = bass.Bass("TRN2", target_bir_lowering=False)
    S = Sched()
    kind_dbg = "ExternalOutput" if debug else "Internal"

    def din(name, shape, dt=F32):
        return nc.dram_tensor(name, shape, dt, kind="ExternalInput").ap()

    x_d = din("x", [NT, D])
    wbig_d = din("wbig", [D, WBIG_COLS])
    wr_d = din("wr", [128, 8 * 16])
    cw_d = din("cw", [128, 24])
    gb_d = din("gb", [128, 6 * D])
    sink_d = din("sinkb", [128, 16])
    emat_d = din("emat", [128, 3 * 16 * 128])
    ident_d = din("ident", [128, 128])
    ones_d = din("ones16", [16, 16])
    offs_d = din("offs", [64, 1])
    wg_d = din("w_gate", [NEXP, D, DFF])
    wu_d = din("w_up", [NEXP, D, DFF])
    wd_d = din("w_down", [NEXP, DFF, D])
    out_d = nc.dram_tensor("out", [NT, D], F32, kind="ExternalOutput").ap()
    xn_d = nc.dram_tensor("xn_s", [NT, D], F32, kind="Internal").ap()
    x1b_d = nc.dram_tensor("x1b_s", [NT, D], BF16, kind=kind_dbg).ap()
    acc_d = nc.dram_tensor("acc_s", [NT, D], F32, kind=kind_dbg).ap()
    aff_d = nc.dram_tensor("aff_s", [64, SEQ], F32, kind=kind_dbg).ap()
    if debug:
        dbg_val = nc.dram_tensor("dbg_val", [128, 128], F32, kind="ExternalOutput").ap()
        dbg_idx = nc.dram_tensor("dbg_idx", [128, 128], I32, kind="ExternalOutput").ap()

    ctx = ExitStack()
    with ctx:
        def sb(name, shape, dt):
            return ctx.enter_context(nc.sbuf_tensor("s_" + name, shape, dt))

        ctx.enter_context(nc.allow_low_precision("bf16 matmul operands, fp32 accumulate"))
        xnT_t = sb("xnT", [128, 16384], BF16)
        bufQ_t = sb("bufQ", [128, 16384], BF16)
        bufY_t = sb("bufY", [128, 16384], BF16)
        kv_t = sb("kv", [128, 4160], BF16)
        kTm_t = sb("kTm", [128, 8192], BF16)
        wsl_t = sb("wsl", [128, 4, 4096], BF16)
        W_t = sb("W", [128, 6144], F32)
        gb01 = sb("gb01", [128, 4, D], F32)
        emat = sb("emat", [128, 3, 16, 128], BF16)
        identb = sb("identb", [128, 128], BF16)
        identf = sb("identf", [128, 128], F32)
        wr_t = sb("wr", [128, 8, 16], F32)
        cw_t = sb("cw", [128, 8, 3], F32)
        esink = sb("esink", [128, 16], F32)
        ones16 = sb("ones16", [16, 16], F32)
        offs_t = sb("offs", [64, 1], F32)
        st_t = sb("stats", [128, 8, 12], F32)
        mv_t = sb("mv", [128, 8, 2], F32)
        rs_t = sb("rstd", [128, 8, 2], F32)
        den_t = sb("den", [128, 2, 32], F32)
        ps_t = ctx.enter_context(nc.psum_tensor("p_ps", [128, 8, 512], F32))

        xnT = xnT_t[:, :].rearrange("p (c t) -> p c t", c=8)
        bufQ = bufQ_t[:, :].rearrange("p (c t) -> p c t", c=8)
        qv = bufQ_t[:, :].rearrange("p (n c t) -> p n c t", n=16, c=8)

        def kQq(n, c0, c1):
            return bk("bufQ", n * 1024 + c0 * 128, n * 1024 + c1 * 128, 512)
        bufY = bufY_t[:, :].rearrange("p (c t) -> p c t", c=8)
        kTm = kTm_t[:, :].rearrange("p (g t) -> p g t", g=4)
        vext = kv_t[:, :].rearrange("p (t g e) -> p t g e", t=16, g=4)

        def kxnT(c0, c1, t0, t1):
            ks = []
            for c in range(c0, c1):
                ks += bk("xnT", c * 2048 + t0, c * 2048 + t1, 512)
            return ks

        def kQ(c0, c1, t0, t1):
            ks = []
            for c in range(c0, c1):
                ks += bk("bufQ", c * 2048 + t0, c * 2048 + t1, 512)
            return ks

        def kY(c0, c1, t0, t1):
            ks = []
            for c in range(c0, c1):
                ks += bk("bufY", c * 2048 + t0, c * 2048 + t1, 512)
            return ks

        def kkT(g, t0, t1):
            return bk("kTm", g * 2048 + t0, g * 2048 + t1, 128)

        def kV(t):
            return bk("kv", t * 260, (t + 1) * 260, 128)

        def Wf(off, n):
            return W_t[:, off:off + n]

        def Wb(off, n):
            return W_t[:, off:off + n].bitcast(BF16)

        def kW(off, n):
            return bk("W", off, off + n, 128)

        def psb(b):
            return ps_t[:, b, :]

        HB = [(0, 7), (7, 14), (14, 16)]

        def kps(b, nb=1):
            ks = []
            for i in range(b, b + nb):
                ks.append(("ps", i))
                if 4 <= i <= 6:
                    ks += [("psO", h) for h in range(*HB[i - 4])]
            return ks

        ring = {"b": 0, "c": 0}

        def nbank():
            b = ring["b"]
            ring["b"] = (b + 1) % 4
            return b

        wsem = [S.dsem("w%d" % i) for i in range(4)]
        pieces = []
        for s in range(NSEQ):
            pieces += [("big", OQ, 512), ("big", OQ + 512, 512), ("big", OKV, 512)]
            pieces += [("big", OE1 + 512 * i, 512) for i in range(4)]
            pieces += [("big", OCONV + 384 * c, 384) for c in range(8)]
            pieces += [("big", OE2 + 512 * i, 512) for i in range(4)]
            pieces += [("big", OWO, 512), ("big", OWO + 512, 512)]
        NP1 = len(pieces)
        for e in range(NEXP):
            for fg in range(4):
                pieces += [("g", e, fg), ("u", e, fg)]
        pstate = {"issued": 0}

        def piece_issue(i):
            p = pieces[i]
            slot = i % 4
            dst = wsl_t[:, slot, :]
            if p[0] == "big":
                _, c0, n = p
                src = wbig_d[:, c0:c0 + n].rearrange("(k p) f -> p k f", p=128)
                d = dst[:, 0:8 * n].rearrange("p (k f) -> p k f", k=8)
            else:
                wsrc = wg_d if p[0] == "g" else wu_d
                src = wsrc[p[1], :, p[2] * 512:(p[2] + 1) * 512].rearrange("(k p) f -> p k f", p=128)
                d = dst.rearrange("p (k f) -> p k f", k=8)
            S.add("pool", lambda e, sem, d=d, src=src: e.dma_start(out=d, in_=src).then_inc(sem, 16),
                  w=[("wsl", slot)], dma=wsem[slot], c=1.5, lat=12.0)

        def want(i, ahead=2):
            tgt = min(len(pieces), i + ahead + 1)
            while pstate["issued"] < tgt:
                piece_issue(pstate["issued"])
                pstate["issued"] += 1

        def pslot(i, n):
            return wsl_t[:, i % 4, 0:8 * n].rearrange("p (k f) -> p k f", k=8)

        csem = S.dsem("const")
        csem2 = S.dsem("constsw")

        cops = []

        def cload(eng, dst, src, key):
            cops.append(S.add(eng, lambda e, sem: e.dma_start(out=dst, in_=src).then_inc(sem, 16), w=[key],
                              dma=(csem if eng == "sp" else csem2)))

        cload("sp", gb01[:, :, :], gb_d[:, 0:4 * D].rearrange("p (a d) -> p a d", a=4), "gb01")
        cload("sp", identf[:, :], ident_d, "identf")
        cload("sp", wr_t[:, :, :], wr_d.rearrange("p (k e) -> p k e", k=8), "wr")
        cload("sp", cw_t[:, :, :], cw_d.rearrange("p (c w) -> p c w", c=8), "cw")
        cload("sp", esink[:, :], sink_d, "esink")
        cload("sp", ones16[:, :], ones_d, "ones16")
        cload("sp", offs_t[:, :], offs_d, "offs")
        cload("pool", identb[:, :], ident_d, "identb")
        cload("pool", emat[:, :, :, :], emat_d.rearrange("p (a h t) -> p a h t", a=3, h=16), "emat")
        for o in cops:
            o.val = o.sem.count
        S.add("act", lambda e: e.activation(out=esink[:, :], in_=esink[:, :], func=AF.Exp), r=["esink"], w=["esink"])
        S.add("pool", lambda e: e.memset(kv_t[:, :], 1.0), w=bk("kv", 0, 4160, 128))
        S.add("pool", lambda e: e.memset(kTm_t[:, :], 0.0), w=bk("kTm", 0, 8192, 128))

        lnc = {"i": 0}

        def layer_norm(src_ap, src_keys, dst_ap, dst_keys, g_ap, b_ap, gbkey, eps=EPS, act_stats=None, style="old"):
            i = lnc["i"] % 8
            lnc["i"] += 1
            st = st_t[:, i, :]
            kst, kmv, krs = ("st", i), ("mv", i), ("rs", i)
            if style != "act":
                S.add("dve", lambda e: e.bn_stats(out=st[:, 0:6], in_=src_ap[:, 0:512]), r=src_keys, w=[(kst, 0)], c=0.65)
                S.add("dve", lambda e: e.bn_stats(out=st[:, 6:12], in_=src_ap[:, 512:1024]), r=src_keys, w=[(kst, 1)], c=0.65)
                S.add("dve", lambda e: e.bn_aggr(out=mv_t[:, i, :], in_=st), r=[(kst, 0), (kst, 1)], w=[kmv], c=0.2)
            else:
                junk, kjunk = act_stats
                S.add("act", lambda e: e.activation(out=src_ap, in_=src_ap, func=AF.Identity, accum_out=st[:, 0:1]),
                      r=src_keys, w=src_keys + [(kst, 8)], c=1.0)
                S.add("act", lambda e: e.activation(out=junk, in_=src_ap, func=AF.Square, accum_out=st[:, 1:2]),
                      r=src_keys, w=kjunk + [(kst, 9)], c=1.0)
                S.add("act", lambda e: e.activation(out=st[:, 4:5], in_=st[:, 5:6], func=AF.Copy),
                      r=[(kst, 8), (kst, 9)], w=[(kst, 0), (kst, 1)], c=0.25)
                S.add("dve", lambda e: e.tensor_scalar(out=mv_t[:, i, 0:1], in0=st[:, 0:1], scalar1=1.0 / D, scalar2=None, op0=ALU.mult),
                      r=[(kst, 0)], w=[(kmv, 0)], c=0.15)
                S.add("dve", lambda e: e.tensor_tensor(out=st[:, 2:3], in0=mv_t[:, i, 0:1], in1=mv_t[:, i, 0:1], op=ALU.mult),
                      r=[(kmv, 0)], w=[(kst, 2)], c=0.15)
                S.add("dve", lambda e: e.scalar_tensor_tensor(out=mv_t[:, i, 1:2], in0=st[:, 1:2], scalar=1.0 / D, in1=st[:, 2:3],
                                                              op0=ALU.mult, op1=ALU.subtract),
                      r=[(kst, 1), (kst, 2), (kmv, 0)], w=[kmv], c=0.15)
            S.add("act", lambda e: e.activation(out=rs_t[:, i, 0:1], in_=mv_t[:, i, 1:2], func=AF.Sqrt, bias=eps, scale=1.0),
                  r=[kmv], w=[(krs, 0)], c=0.3)
            S.add("dve", lambda e: e.reciprocal(out=rs_t[:, i, 0:1], in_=rs_t[:, i, 0:1]), r=[(krs, 0)], w=[(krs, 0)], c=0.15)
            if style == "old":
                S.add("dve", lambda e: e.scalar_tensor_tensor(out=rs_t[:, i, 1:2], in0=mv_t[:, i, 0:1], scalar=-1.0,
                                                              in1=rs_t[:, i, 0:1], op0=ALU.mult, op1=ALU.mult),
                      r=[kmv, (kmv, 0), (krs, 0)], w=[(krs, 1)], c=0.12)
                S.add("act", lambda e: e.activation(out=dst_ap, in_=src_ap, func=AF.Identity, scale=rs_t[:, i, 0:1],
                                                    bias=rs_t[:, i, 1:2]), r=src_keys + [(krs, 0), (krs, 1)], w=dst_keys, c=1.0)
                S.add("dve", lambda e: e.tensor_tensor(out=dst_ap, in0=dst_ap, in1=g_ap, op=ALU.mult),
                      r=dst_keys + [gbkey], w=dst_keys, c=1.2)
                S.add("dve", lambda e: e.tensor_tensor(out=dst_ap, in0=dst_ap, in1=b_ap, op=ALU.add),
                      r=dst_keys + [gbkey], w=dst_keys, c=1.2)
            else:
                S.add("dve", lambda e: e.scalar_tensor_tensor(out=dst_ap, in0=src_ap, scalar=mv_t[:, i, 0:1], in1=g_ap,
                                                              op0=ALU.subtract, op1=ALU.mult),
                      r=src_keys + [kmv, (kmv, 0), gbkey], w=dst_keys, c=1.2)
                S.add("dve", lambda e: e.scalar_tensor_tensor(out=dst_ap, in0=dst_ap, scalar=rs_t[:, i, 0:1], in1=b_ap,
                                                              op0=ALU.mult, op1=ALU.add),
                      r=dst_keys + [(krs, 0), gbkey], w=dst_keys, c=1.2)

        def mm_group(out_ap, pairs, rkeys, wkeys, c=None):
            n = len(pairs)
            if c is None:
                c = 0.08 + n * 0.256

            def fn(e):
                ins = None
                for i, (l, r) in enumerate(pairs):
                    ins = e.matmul(out_ap, l, r, start=(i == 0), stop=(i == n - 1))
                return ins
            S.add("pe", fn, r=rkeys, w=wkeys, c=c)

        def transposes8(src_ap, src_keys, bank, ident, nbk=1, dt=BF16):
            if dt == BF16:
                dst = ps_t[:, bank, :].bitcast(BF16).rearrange("p (c t) -> p c t", c=8)
            else:
                dst = ps_t[:, bank:bank + 2, :].rearrange("p a (c t) -> p (a c) t", c=4)

            def fn(e):
                ins = None
                for c in range(8):
                    ins = e.transpose(dst[:, c, :], src_ap[:, c * 128:(c + 1) * 128], ident)
                return ins
            S.add("pe", fn, r=src_keys, w=kps(bank, nbk), c=(0.7 if dt == BF16 else 2.2))
            return dst

        xin_sem = [S.dsem("xin%d" % i) for i in range(4)]
        xns_sem = [S.dsem("xns%d" % i) for i in range(4)]
        xnr_sem = [S.dsem("xnr%d" % i) for i in range(4)]
        x1s_sem = [S.dsem("x1s%d" % i) for i in range(2)]
        acs_sem = [S.dsem("acs%d" % i) for i in range(4)]
        afs_sem = S.dsem("afs")

        pi = 0
        for s in range(NSEQ):
            T0 = s * SEQ
            want(pi, 2)
            for t in range(NT16):
                sl = t % 4
                r0 = T0 + t * 128
                if sl < 2:
                    xin = Wf(sl * 1024, 1024)
                    kxin = kW(sl * 1024, 1024)
                else:
                    xo = 10240 + (sl - 2) * 2048
                    xin = bufY_t[:, xo:xo + 2048].bitcast(F32)
                    kxin = bk("bufY", xo, xo + 2048, 512)
                xn16 = bufY_t[:, (t % 2) * 1024:(t % 2 + 1) * 1024]
                kxn16 = bk("bufY", (t % 2) * 1024, (t % 2 + 1) * 1024, 512)
                S.add("sp", lambda e, sem, xin=xin, r0=r0: e.dma_start(out=xin, in_=x_d[r0:r0 + 128, :]).then_inc(sem, 16),
                      w=kxin, dma=xin_sem[sl], lat=4.0)
                layer_norm(xin, kxin, xin, kxin, gb01[:, 0, :], gb01[:, 1, :], "gb01", style=cfg["lnA"])
                S.add("act", lambda e, a=xn16, b=xin: e.activation(out=a, in_=b, func=AF.Copy), r=kxin, w=kxn16, c=1.0)
                S.add("sp", lambda e, sem, a=xin, r0=r0: e.dma_start(out=xn_d[r0:r0 + 128, :], in_=a).then_inc(sem, 16),
                      r=kxin, w=[("xn_d", s, t)], dma=xns_sem[sl], lat=4.0)
                pt = transposes8(xn16, kxn16 + ["identb"], 7, identb[:, :])
                S.add("act", lambda e, pt=pt, t=t: e.activation(out=xnT[:, :, t * 128:(t + 1) * 128], in_=pt, func=AF.Copy),
                      r=kps(7), w=kxnT(0, 8, t * 128, (t + 1) * 128), c=1.0)

            for qp in range(2):
                want(pi, 2)
                wv = pslot(pi, 512)
                for cc in range(4):
                    c = qp * 4 + cc
                    for tg in range(4):
                        b = nbank()
                        mm_group(psb(b), [(wv[:, k, cc * 128:(cc + 1) * 128], xnT[:, k, tg * 512:(tg + 1) * 512]) for k in range(8)],
                                 [("wsl", pi % 4)] + kxnT(0, 8, tg * 512, (tg + 1) * 512), kps(b))
                        S.add("act", lambda e, b=b, c=c, tg=tg: e.activation(
                            out=qv[:, tg * 4:(tg + 1) * 4, c, :], in_=psb(b).rearrange("p (n t) -> p n t", n=4),
                            func=AF.Identity, scale=0.125),
                            r=kps(b), w=sum([kQq(tg * 4 + i, c, c + 1) for i in range(4)], []), c=0.65)
                pi += 1
            want(pi, 2)
            wv = pslot(pi, 512)
            for j in range(2):
                for tg in range(4):
                    b = nbank()
                    mm_group(psb(b), [(wv[:, k, j * 128:(j + 1) * 128], xnT[:, k, tg * 512:(tg + 1) * 512]) for k in range(8)],
                             [("wsl", pi % 4)] + kxnT(0, 8, tg * 512, (tg + 1) * 512), kps(b))
                    S.add("dve", lambda e, b=b, j=j, tg=tg: e.tensor_copy(kTm[0:64, j, tg * 512:(tg + 1) * 512], ps_t[0:64, b, :]),
                          r=kps(b), w=kkT(j, tg * 512, (tg + 1) * 512), c=0.65)
                    S.add("dve", lambda e, b=b, j=j, tg=tg: e.tensor_copy(kTm[64:128, j + 2, tg * 512:(tg + 1) * 512],
                                                                          ps_t[64:128, b, :]),
                          r=kps(b), w=kkT(j + 2, tg * 512, (tg + 1) * 512), c=0.65)
            for t in range(NT16):
                b = nbank()
                mm_group(ps_t[:, b, 0:256], [(xnT[:, k, t * 128:(t + 1) * 128], wv[:, k, 256:512]) for k in range(8)],
                         [("wsl", pi % 4)] + kxnT(0, 8, t * 128, (t + 1) * 128), kps(b))
                S.add("dve", lambda e, b=b, t=t: e.tensor_copy(vext[:, t, :, 0:64],
                                                             ps_t[:, b, 0:256].rearrange("p (g e) -> p g e", g=4)),
                      r=kps(b), w=kV(t))
            pi += 1

            want(pi, 2)
            def o_ap(h):
                bnk = 4 + h // 7
                o = (h % 7) * 65
                return ps_t[:, bnk, o:o + 65]

            for n in range(NT16):
                kbs = [kb for kb in (n - 1, n, n + 1) if 0 <= kb < NT16]
                for g in range(4):
                    r0 = 64 * (g // 2)
                    kc = g % 2
                    c0 = (g % 2) * 4
                    pts = []
                    for kbi, kb in enumerate(kbs):
                        typ = kb - n + 1
                        b = nbank()
                        eb = Wb(kbi * 256, 256)
                        keb = kW(kbi * 256, 256)
                        pslot_i = (g % 2) * 3 + kbi
                        pT = Wb(1536 + pslot_i * 256, 256)
                        kpT = kW(1536 + pslot_i * 256, 256)
                        pts.append((pT, kpT))
                        mm_group(psb(b).rearrange("p (h t) -> p h t", h=4), [(kTm[:, g, kb * 128:(kb + 1) * 128],
                                           qv[:, n, c0:c0 + 4, :])],
                                 kkT(g, kb * 128, (kb + 1) * 128) + kQq(n, c0, c0 + 4), kps(b), c=0.3)
                        S.add("act", lambda e, eb=eb, b=b: e.activation(out=eb, in_=psb(b), func=AF.Exp), r=kps(b), w=keb)
                        S.add("dve", lambda e, pT=pT, eb=eb, typ=typ, g=g: e.tensor_tensor(
                            out=pT.rearrange("p (h t) -> p h t", h=4), in0=eb.rearrange("p (h t) -> p h t", h=4),
                            in1=emat[:, typ, 4 * g:4 * g + 4, :], op=ALU.mult), r=keb + ["emat"], w=kpT)
                    for r in range(4):
                        h = 4 * g + r
                        mm_group(o_ap(h), [(pts[kbi][0][:, r * 128:(r + 1) * 128], vext[:, kb, g, :]) for kbi, kb in enumerate(kbs)],
                                 sum([pts[kbi][1] for kbi in range(len(kbs))], []) + sum([kV(kb) for kb in kbs], []),
                                 [("psO", h)], c=0.25)
                di = n % 2
                den = den_t[:, di, 0:16]
                rden = den_t[:, di, 16:32]
                yat = Wb(3072 + di * 512, 512)
                kyat = kW(3072 + di * 512, 512)
                for bi, (h0, h1) in enumerate(HB):
                    nh = h1 - h0
                    ov = ps_t[:, 4 + bi, 0:nh * 65].rearrange("p (h e) -> p h e", h=nh)
                    S.add("dve", lambda e, ov=ov, h0=h0, h1=h1, den=den: e.tensor_tensor(
                        out=den[:, h0:h1], in0=ov[:, :, 64], in1=esink[:, h0:h1], op=ALU.add),
                        r=[("psO", h) for h in range(h0, h1)] + ["esink"], w=[("den", di, bi)], c=0.15)
                S.add("dve", lambda e, den=den, rden=rden: e.reciprocal(out=rden, in_=den),
                      r=[("den", di, bi) for bi in range(3)], w=[("rden", di)])
                for bi, (h0, h1) in enumerate(HB):
                    nh = h1 - h0
                    ov = ps_t[:, 4 + bi, 0:nh * 65].rearrange("p (h e) -> p h e", h=nh)
                    S.add("dve", lambda e, ov=ov, h0=h0, h1=h1, nh=nh, rden=rden, yat=yat: e.tensor_tensor(
                        out=yat[:, h0 * 64:h1 * 64].rearrange("p (h e) -> p h e", h=nh), in0=ov[:, :, 0:64],
                        in1=rden[:, h0:h1].unsqueeze(2).to_broadcast([128, nh, 64]), op=ALU.mult),
                        r=[("psO", h) for h in range(h0, h1)] + [("rden", di)], w=kyat, c=0.6)
                pt = transposes8(yat, kyat + ["identb"], 7, identb[:, :])
                S.add("act", lambda e, pt=pt, n=n: e.activation(out=bufY[:, :, n * 128:(n + 1) * 128], in_=pt, func=AF.Copy),
                      r=kps(7), w=kY(0, 8, n * 128, (n + 1) * 128))

            for ep in range(4):
                want(pi, 2)
                wv = pslot(pi, 512)
                for cc in range(2):
                    c = ep * 2 + cc
                    for tg in range(4):
                        b1 = nbank()
                        b2 = nbank()
                        tsl = slice(tg * 512, (tg + 1) * 512)
                        mm_group(psb(b1), [(wv[:, k, cc * 256:cc * 256 + 128], xnT[:, k, tsl]) for k in range(8)],
                                 [("wsl", pi % 4)] + kxnT(0, 8, tg * 512, (tg + 1) * 512), kps(b1))
                        mm_group(psb(b2), [(wv[:, k, cc * 256 + 128:cc * 256 + 256], bufY[:, k, tsl]) for k in range(8)],
                                 [("wsl", pi % 4)] + kY(0, 8, tg * 512, (tg + 1) * 512), kps(b2))
                        si = (c * 4 + tg) % 2
                        sg = Wf(si * 512, 512)
                        ksg = kW(si * 512, 512)
                        S.add("act", lambda e, sg=sg, b1=b1: e.activation(out=sg, in_=psb(b1), func=AF.Sigmoid), r=kps(b1), w=ksg)
                        S.add("dve", lambda e, sg=sg, b2=b2, c=c, tsl=tsl: e.tensor_tensor(out=bufQ[:, c, tsl], in0=psb(b2), in1=sg,
                                                                                         op=ALU.mult),
                              r=kps(b2) + ksg, w=kQ(c, c + 1, tg * 512, (tg + 1) * 512))
                pi += 1

            UO = 1024
            for c in range(8):
                want(pi, 2)
                wv = pslot(pi, 384)
                u = Wf(UO, 2050)
                S.add("dve", lambda e, u=u: e.memset(u[:, 0:1], 0.0), w=kW(UO, 1), c=0.1)
                S.add("dve", lambda e, u=u: e.memset(u[:, 2049:2050], 0.0), w=kW(UO + 2049, 1), c=0.1)
                cbank = {}

                def conv_out(tgi, c=c, u=u, cbank=cbank):
                    j0 = tgi * 512
                    yi = tgi % 2
                    yt = Wf(3200 + yi * 512, 512)
                    kyt = kW(3200 + yi * 512, 512)
                    bC = cbank[tgi]
                    ku = kW(UO + j0, 514)
                    S.add("act", lambda e: e.activation(out=yt, in_=u[:, j0:j0 + 512], func=AF.Identity, scale=cw_t[:, c, 0:1]),
                          r=ku + ["cw"], w=kyt, c=0.65)
                    S.add("dve", lambda e: e.scalar_tensor_tensor(out=yt, in0=u[:, j0 + 1:j0 + 513], scalar=cw_t[:, c, 1:2], in1=yt,
                                                                  op0=ALU.mult, op1=ALU.add), r=ku + ["cw"] + kyt, w=kyt, c=0.65)
                    S.add("dve", lambda e: e.scalar_tensor_tensor(out=yt, in0=u[:, j0 + 2:j0 + 514], scalar=cw_t[:, c, 2:3], in1=yt,
                                                                  op0=ALU.mult, op1=ALU.add), r=ku + ["cw"] + kyt, w=kyt, c=0.65)
                    S.add("dve", lambda e: e.tensor_tensor(out=bufY[:, c, j0:j0 + 512], in0=yt, in1=psb(bC), op=ALU.mult),
                          r=kyt + kps(bC), w=kY(c, c + 1, j0, j0 + 512), c=0.65)

                for tg in range(4):
                    tsl = slice(tg * 512, (tg + 1) * 512)
                    bA, bB = nbank(), nbank()
                    bC = 4 + (ring["c"] % 3)
                    ring["c"] += 1
                    cbank[tg] = bC
                    rk = [("wsl", pi % 4)] + kxnT(0, 8, tg * 512, (tg + 1) * 512)
                    mm_group(psb(bA), [(wv[:, k, 0:128], xnT[:, k, tsl]) for k in range(8)], rk, kps(bA))
                    mm_group(psb(bB), [(wv[:, k, 128:256], xnT[:, k, tsl]) for k in range(8)], rk, kps(bB))
                    mm_group(psb(bC), [(wv[:, k, 256:384], xnT[:, k, tsl]) for k in range(8)], rk, kps(bC))
                    ci = tg % 2
                    ccs = Wf(ci * 512, 512)
                    kccs = kW(ci * 512, 512)
                    S.add("act", lambda e, ccs=ccs, bA=bA: e.activation(out=ccs, in_=psb(bA), func=AF.Copy), r=kps(bA), w=kccs, c=0.65)
                    S.add("dve", lambda e, ccs=ccs, bB=bB, tg=tg, u=u: e.tensor_tensor(out=u[:, 1 + tg * 512:1 + (tg + 1) * 512],
                                                                                     in0=psb(bB), in1=ccs, op=ALU.mult),
                          r=kps(bB) + kccs, w=kW(UO + 1 + tg * 512, 512), c=0.65)
                    if tg >= 1:
                        conv_out(tg - 1)
                conv_out(3)
                pi += 1

            for ep in range(4):
                want(pi, 2)
                wv = pslot(pi, 512)
                for cc in range(2):
                    c = ep * 2 + cc
                    for tg in range(4):
                        b1 = nbank()
                        b2 = nbank()
                        tsl = slice(tg * 512, (tg + 1) * 512)
                        mm_group(psb(b1), [(wv[:, k, cc * 256:cc * 256 + 128], xnT[:, k, tsl]) for k in range(8)],
                                 [("wsl", pi % 4)] + kxnT(0, 8, tg * 512, (tg + 1) * 512), kps(b1))
                        mm_group(psb(b2), [(wv[:, k, cc * 256 + 128:cc * 256 + 256], bufY[:, k, tsl]) for k in range(8)],
                                 [("wsl", pi % 4)] + kY(0, 8, tg * 512, (tg + 1) * 512), kps(b2))
                        si = (c * 4 + tg) % 2
                        sg = Wf(si * 512, 512)
                        ksg = kW(si * 512, 512)
                        t2 = Wf(1024 + si * 512, 512)
                        kt2 = kW(1024 + si * 512, 512)
                        S.add("act", lambda e, sg=sg, b1=b1: e.activation(out=sg, in_=psb(b1), func=AF.Sigmoid), r=kps(b1), w=ksg, c=0.65)
                        S.add("dve", lambda e, sg=sg, b2=b2, t2=t2: e.tensor_tensor(out=t2, in0=psb(b2), in1=sg, op=ALU.mult),
                              r=kps(b2) + ksg, w=kt2, c=0.65)
                        S.add("dve", lambda e, t2=t2, c=c, tsl=tsl: e.tensor_tensor(out=bufQ[:, c, tsl], in0=t2, in1=bufQ[:, c, tsl],
                                                                                  op=ALU.add),
                              r=kt2 + kQ(c, c + 1, tg * 512, (tg + 1) * 512), w=kQ(c, c + 1, tg * 512, (tg + 1) * 512), c=0.65)
                pi += 1

            want(pi, 2)
            want(pi + 1, 2)
            wA = pslot(pi, 512)
            wB = pslot(pi + 1, 512)
            kwAB = [("wsl", pi % 4), ("wsl", (pi + 1) % 4)]
            for t in range(NT16):
                r0 = T0 + t * 128
                zi = t % cfg["nzF"]
                zb = Wf(2048 + zi * 1024, 1024)
                kzb = kW(2048 + zi * 1024, 1024)
                xi = t % 2
                x1b = bufY_t[:, 2048 + xi * 1024:2048 + (xi + 1) * 1024]
                kx1b = bk("bufY", 2048 + xi * 1024, 2048 + (xi + 1) * 1024, 512)
                x1T = bufY_t[:, 4096 + xi * 2048:4096 + (xi + 1) * 2048].bitcast(F32).rearrange("p (c t) -> p c t", c=8)
                kx1T = bk("bufY", 4096 + xi * 2048, 4096 + (xi + 1) * 2048, 512)
                ex = bufY_t[0:16, 8192:8448].bitcast(F32)
                rsm = bufY_t[0:16, 8704:8960].bitcast(F32)
                affo = bufY_t[0:16, 9216:9472].bitcast(F32)
                kex, krsm, kaffo = [("bufY", 16)], [("bufY", 17)], [("bufY", 18)]
                S.add("sp", lambda e, sem, zb=zb, r0=r0: e.dma_start(out=zb, in_=xn_d[r0:r0 + 128, :]).then_inc(sem, 16),
                      r=[("xn_d", s, t)], w=kzb, dma=xnr_sem[zi], lat=4.0)
                tok = slice(t * 128, (t + 1) * 128)
                hb = 4 if t % 2 == 0 else 6
                mm_group(psb(hb), [(bufQ[:, k, tok], wA[:, k, :]) for k in range(8)], kwAB + kQ(0, 8, t * 128, (t + 1) * 128), kps(hb))
                mm_group(psb(hb + 1), [(bufQ[:, k, tok], wB[:, k, :]) for k in range(8)], kwAB + kQ(0, 8, t * 128, (t + 1) * 128),
                         kps(hb + 1))
                S.add("dve", lambda e, zb=zb, hb=hb: e.scalar_tensor_tensor(
                    out=zb.rearrange("p (a f) -> p a f", a=2), in0=zb.rearrange("p (a f) -> p a f", a=2), scalar=ALPHA,
                    in1=ps_t[:, hb:hb + 2, :], op0=ALU.mult, op1=ALU.add), r=kzb + kps(hb, 2), w=kzb, c=1.2)
                layer_norm(zb, kzb, zb, kzb, gb01[:, 2, :], gb01[:, 3, :], "gb01", act_stats=(x1b, kx1b), style=cfg["lnF"])
                S.add("act", lambda e, x1b=x1b, zb=zb: e.activation(out=x1b, in_=zb, func=AF.Copy), r=kzb, w=kx1b, c=1.0)
                S.add("sp", lambda e, sem, x1b=x1b, r0=r0: e.dma_start(out=x1b_d[r0:r0 + 128, :], in_=x1b).then_inc(sem, 16),
                      r=kx1b, w=[("x1b_d", s, t)], dma=x1s_sem[xi], lat=3.0)
                S.add("sp", lambda e, sem, zb=zb, r0=r0: e.dma_start(out=acc_d[r0:r0 + 128, :], in_=zb).then_inc(sem, 16),
                      r=kzb, w=[("acc_d", s, t)], dma=acs_sem[zi], lat=4.0)
                tb = 2 if t % 2 == 0 else 0
                pt = transposes8(zb, kzb + ["identf"], tb, identf[:, :], nbk=2, dt=F32)
                S.add("act", lambda e, pt=pt, x1T=x1T: e.activation(out=x1T, in_=pt, func=AF.Copy), r=kps(tb, 2), w=kx1T, c=1.0)
                mm_group(ps_t[0:16, tb, 0:128], [(wr_t[:, k, :], x1T[:, k, :]) for k in range(8)], kx1T + ["wr"], kps(tb), c=0.6)
                S.add("act", lambda e, ex=ex, tb=tb: e.activation(out=ex, in_=ps_t[0:16, tb, 0:128], func=AF.Exp), r=kps(tb), w=kex,
                      c=0.3)
                mm_group(ps_t[0:16, tb + 1, 0:128], [(ones16[:, :], ex)], kex + ["ones16"], kps(tb + 1), c=0.2)
                S.add("dve", lambda e, rsm=rsm, tb=tb: e.reciprocal(out=rsm, in_=ps_t[0:16, tb + 1, 0:128]), r=kps(tb + 1), w=krsm,
                      c=0.25)
                S.add("dve", lambda e, affo=affo, ex=ex, rsm=rsm: e.tensor_tensor(out=affo, in0=ex, in1=rsm, op=ALU.mult),
                      r=kex + krsm, w=kaffo, c=0.25)
                S.add("sp", lambda e, sem, affo=affo, t=t, s=s: e.dma_start(out=aff_d[s * 16:(s + 1) * 16, t * 128:(t + 1) * 128],
                                                                           in_=affo).then_inc(sem, 16),
                      r=kaffo, w=[("aff_d",)], dma=afs_sem, lat=3.0)
            pi += 2
        assert pi == NP1

        NR = NSEQ * 16
        aff_all = W_t[0:NR, 0:2048]
        work = W_t[0:NR, 2048:4096]
        vals = W_t[0:NR, 4096:4352]
        idxu = W_t[0:NR, 4352:4608].bitcast(U32)
        idxf = W_t[0:NR, 4608:4864]
        valT = W_t[:, 4864:4992].rearrange("p (a c) -> p a c", a=2)
        idxT = W_t[:, 4992:5120].bitcast(I32).rearrange("p (a c) -> p a c", a=2)
        tk_sem = S.dsem("tk")
        S.add("sp", lambda e, sem: e.dma_start(out=aff_all, in_=aff_d[0:NR, :]).then_inc(sem, 16),
              r=[("aff_d",)], w=kW(0, 2048) + kW(4096, 1024), dma=tk_sem)
        for it in range(CAP // 8):
            src = aff_all if it == 0 else work
            ksrc = kW(0, 2048) if it == 0 else kW(2048, 2048)
            v8 = vals[:, it * 8:(it + 1) * 8]
            S.add("dve", lambda e, v8=v8, src=src: e.max(out=v8, in_=src), r=ksrc, w=[("v8", it)], c=2.3,
                  soft=(cfg["soft_topk"] and it > 0))
            S.add("dve", lambda e, v8=v8, src=src, it=it: e.max_index(out=idxu[:, it * 8:(it + 1) * 8], in_max=v8, in_values=src),
                  r=ksrc + [("v8", it)], w=[("i8", it)], c=2.3)
            if it < CAP // 8 - 1:
                S.add("dve", lambda e, v8=v8, src=src: e.match_replace(out=work, in_to_replace=v8, in_values=src, imm_value=-1.0),
                      r=ksrc + [("v8", it)], w=kW(2048, 2048), c=2.3, soft=cfg["soft_topk"])
        allv = [("v8", it) for it in range(CAP // 8)]
        alli = [("i8", it) for it in range(CAP // 8)]
        S.add("dve", lambda e: e.tensor_copy(idxf, idxu), r=alli, w=[("idxf",)])
        S.add("dve", lambda e: e.tensor_scalar(out=idxf, in0=idxf, scalar1=offs_t[0:NR, 0:1], scalar2=None, op0=ALU.add),
              r=[("idxf",), "offs"], w=[("idxf",)])

        def tkT(e):
            ins = None
            for half in range(2):
                ins = e.transpose(ps_t[:, 0, half * 64:half * 64 + NR], vals[:, half * 128:(half + 1) * 128], identf[0:NR, 0:NR])
                ins = e.transpose(ps_t[:, 1, half * 64:half * 64 + NR], idxf[:, half * 128:(half + 1) * 128], identf[0:NR, 0:NR])
            return ins
        S.add("pe", tkT, r=allv + [("idxf",), "identf"], w=kps(0, 2))
        S.add("act", lambda e: e.activation(out=valT[:, :, 0:NR], in_=ps_t[:, 0, 0:128].rearrange("p (a c) -> p a c", a=2)[:, :, 0:NR],
                                            func=AF.Identity, scale=1.0 / ALPHA), r=kps(0), w=kW(4864, 128))
        S.add("dve", lambda e: e.tensor_copy(idxT[:, :, 0:NR], ps_t[:, 1, 0:128].rearrange("p (a c) -> p a c", a=2)[:, :, 0:NR]),
              r=kps(1), w=kW(4992, 128))
        if debug:
            dsm = S.dsem("dbg")
            S.add("sp", lambda e, sem: e.dma_start(out=dbg_val, in_=W_t[:, 4864:4992]).then_inc(sem, 16), r=kW(4864, 128), w=[("dbgv",)], dma=dsm)
            S.add("sp", lambda e, sem: e.dma_start(out=dbg_idx, in_=W_t[:, 4992:5120].bitcast(I32)).then_inc(sem, 16), r=kW(4992, 128),
                  w=[("dbgi",)], dma=dsm)
            S.final_waits.append((dsm, 32))

        xg = xnT_t[:, :].rearrange("p (a j d) -> p a j d", a=2, j=8)
        xgT = kTm_t[:, :].rearrange("p (c t) -> p c t", c=8)
        hT = bufQ_t[:, :].rearrange("p (c t) -> p c t", c=16)
        wdv = bufY_t[:, :].rearrange("p (c n) -> p c n", c=16)
        g_sem = [S.dsem("gath%d" % i) for i in range(2)]
        wd_sem = S.dsem("wd")
        sc_sem = [S.dsem("scat%d" % i) for i in range(2)]
        allx1b = [("x1b_d", s, t) for s in range(NSEQ) for t in range(NT16)]

        def kxg(a, j0, j1):
            return bk("xnT", a * 8192 + j0 * 1024, a * 8192 + j1 * 1024, 512)

        def kxgT(j0, j1):
            return [("kTm", c * 8 + jj) for c in range(8) for jj in range(j0, j1)]

        def gather(ex_):
            a = ex_ % 2

            def fn(e, sem):
                for s in range(NSEQ):
                    for half in range(2):
                        col = s * 16 + ex_
                        e.indirect_dma_start(out=xg[:, a, s * 2 + half, :], out_offset=None, in_=x1b_d[:, :],
                                             in_offset=bass.IndirectOffsetOnAxis(ap=idxT[:, half, col:col + 1], axis=0)
                                             ).then_inc(sem, 16)
            S.add("pool", fn, r=allx1b + kW(4992, 128), w=kxg(a, 0, 2 * NSEQ), dma=g_sem[a], ndma=2 * NSEQ, c=2.0 * 2 * NSEQ, lat=2.0 * 2 * NSEQ + 10)

        NJ = 2 * NSEQ
        NSL = NJ * 128
        SH = [(o, min(512, NSL - o)) for o in range(0, NSL, 512)]
        gather(0)
        for ex_ in range(NEXP):
            a = ex_ % 2
            if ex_ + 1 < NEXP:
                gather(ex_ + 1)
            for j in range(NJ):
                pt = transposes8(xg[:, a, j, :], kxg(a, j, j + 1) + ["identb"], 7, identb[:, :])
                eng = "act" if j % 2 == 0 else "dve"
                if eng == "act":
                    S.add("act", lambda e, pt=pt, j=j: e.activation(out=xgT[:, :, j * 128:(j + 1) * 128], in_=pt, func=AF.Copy),
                          r=kps(7), w=kxgT(j, j + 1), c=1.0)
                else:
                    S.add("dve", lambda e, pt=pt, j=j: e.tensor_copy(xgT[:, :, j * 128:(j + 1) * 128], pt),
                          r=kps(7), w=kxgT(j, j + 1), c=1.2)
            for fg in range(4):
                want(pi, 2)
                want(pi + 1, 2)
                wG = pslot(pi, 512)
                wU = pslot(pi + 1, 512)
                for fc in range(4):
                    f = fg * 4 + fc
                    for (so, sn) in SH:
                        bG, bU = nbank(), nbank()
                        rk = kxgT(so // 128, (so + sn) // 128)
                        mm_group(ps_t[:, bG, 0:sn], [(wG[:, k, fc * 128:(fc + 1) * 128], xgT[:, k, so:so + sn]) for k in range(8)],
                                 [("wsl", pi % 4)] + rk, kps(bG))
                        mm_group(ps_t[:, bU, 0:sn], [(wU[:, k, fc * 128:(fc + 1) * 128], xgT[:, k, so:so + sn]) for k in range(8)],
                                 [("wsl", (pi + 1) % 4)] + rk, kps(bU))
                        si = (f + so // 512) % 2
                        sgt = Wf(si * 512, 512)
                        ksgt = kW(si * 512, 512)
                        S.add("act", lambda e, sgt=sgt, bG=bG, sn=sn: e.activation(out=sgt[:, 0:sn], in_=ps_t[:, bG, 0:sn], func=AF.Silu),
                              r=kps(bG), w=ksgt, c=0.65)
                        S.add("dve", lambda e, sgt=sgt, bU=bU, sn=sn, f=f, so=so: e.tensor_tensor(
                            out=hT[:, f, so:so + sn], in0=ps_t[:, bU, 0:sn], in1=sgt[:, 0:sn], op=ALU.mult),
                            r=kps(bU) + ksgt, w=bk("bufQ", f * 1024 + so, f * 1024 + so + sn, 512), c=0.65)
                pi += 2
            def wdl(e, sem, ex_=ex_):
                for q in range(4):
                    e.dma_start(out=wdv[:, 4 * q:4 * q + 4, :],
                                in_=wd_d[ex_, q * 512:(q + 1) * 512, :].rearrange("(c p) n -> p c n", p=128)).then_inc(sem, 16)
            S.add("pool", wdl, w=bk("bufY", 0, 16384, 512), dma=wd_sem, ndma=4, c=4.0, lat=30.0)
            for j in range(NJ):
                s, half = j // 2, j % 2
                col = s * 16 + ex_
                pb = 4 if j % 2 == 0 else 2
                for dh in range(2):
                    mm_group(psb(pb + dh), [(hT[:, f, j * 128:(j + 1) * 128], wdv[:, f, dh * 512:(dh + 1) * 512]) for f in range(16)],
                             [("bufQ", f * 2 + j // 4) for f in range(16)] + [("bufY", f * 2 + dh) for f in range(16)], kps(pb + dh))
                yi = j % 2
                ye = Wf(1024 + yi * 1024, 1024)
                kye = kW(1024 + yi * 1024, 1024)
                S.add("act", lambda e, ye=ye, pb=pb, half=half, col=col: e.activation(
                    out=ye.rearrange("p (a f) -> p a f", a=2), in_=ps_t[:, pb:pb + 2, :], func=AF.Identity,
                    scale=valT[:, half, col:col + 1]), r=kps(pb, 2) + kW(4864, 128), w=kye, c=1.0)
                S.add("pool", lambda e, sem, ye=ye, half=half, col=col: e.indirect_dma_start(
                    out=acc_d[:, :], out_offset=bass.IndirectOffsetOnAxis(ap=idxT[:, half, col:col + 1], axis=0),
                    in_=ye, in_offset=None, compute_op=ALU.add).then_inc(sem, 16),
                    r=kye + kW(4992, 128), w=[("acc_d", s, t) for t in range(NT16)], dma=sc_sem[yi], c=2.0, lat=8.0)

        gb2 = gb01[:, 0:2, :]
        g2_sem = S.dsem("gb2")
        S.add("sp", lambda e, sem: e.dma_start(out=gb2, in_=gb_d[:, 4 * D:6 * D].rearrange("p (a d) -> p a d", a=2)).then_inc(sem, 16),
              w=["gb01"], dma=g2_sem)
        NFS = cfg["nfs"]
        fl_sem = [S.dsem("fl%d" % i) for i in range(NFS)]
        fs_sem = [S.dsem("fs%d" % i) for i in range(NFS)]
        jk = Wb(5120, 512)
        kjk = kW(5120, 512)
        fi = 0
        for s in range(NSEQ):
            for t in range(NT16):
                r0 = s * SEQ + t * 128
                i = fi % NFS
                fi += 1
                fin = Wf(i * 1024, 1024)
                kfin = kW(i * 1024, 1024)
                S.add("sp", lambda e, sem, fin=fin, r0=r0: e.dma_start(out=fin, in_=acc_d[r0:r0 + 128, :]).then_inc(sem, 16),
                      r=[("acc_d", s, t)], w=kfin, dma=fl_sem[i], lat=4.0)
                layer_norm(fin, kfin, fin, kfin, gb2[:, 0, :], gb2[:, 1, :], "gb01", eps=EPS / (ALPHA * ALPHA),
                           act_stats=(jk, kjk), style=cfg["lnFin"])
                S.add("sp", lambda e, sem, fin=fin, r0=r0: e.dma_start(out=out_d[r0:r0 + 128, :], in_=fin).then_inc(sem, 16),
                      r=kfin, w=[("out_d", s, t)], dma=fs_sem[i], lat=4.0)
        for d in fs_sem + acs_sem + sc_sem + x1s_sem + [afs_sem]:
            S.final_waits.append((d, d.count))

        S.reorder()
        block = ctx.enter_context(nc.Block())
        S.emit(nc, block, ctx)
    return nc


def _alibi_table():
    slopes = (2.0 ** (-8.0 * np.arange(1, 17) / 16)).astype(np.float64)
    j = np.arange(128)[:, None]
    i = np.arange(128)[None, :]
    E = np.zeros((128, 3, 16, 128), np.float32)
    for typ, dist in enumerate((128 + i - j, np.abs(i - j), 128 + j - i)):
        valid = dist <= 128
        for h in range(16):
            E[:, typ, h, :] = np.where(valid, np.exp(-slopes[h] * dist), 0.0)
    return E.reshape(128, -1)


def _prep_shared(inp):
    w_in = np.asarray(inp["w_in"], np.float32)[0]
    q, k, v, cb, cc, cx, ga, gc = np.split(w_in, [1024, 1280, 1536, 2560, 3584, 4608, 5632], axis=1)
    wao = np.asarray(inp["w_attn_o"], np.float32)[0]
    wco = np.asarray(inp["w_conv_o"], np.float32)[0]
    wo = np.asarray(inp["w_out"], np.float32)[0]
    cols = []
    for c in range(8):
        cols += [q[:, c * 64:(c + 1) * 64], q[:, (8 + c) * 64:(9 + c) * 64]]
    for g in (0, 2, 1, 3):
        cols.append(k[:, g * 64:(g + 1) * 64])
    cols.append(v)
    for c in range(8):
        sl = slice(c * 128, (c + 1) * 128)
        cols += [cc[:, sl], cx[:, sl], cb[:, sl]]
    for c in range(8):
        sl = slice(c * 128, (c + 1) * 128)
        cols += [ga[:, sl], wao[:, sl]]
    for c in range(8):
        sl = slice(c * 128, (c + 1) * 128)
        cols += [gc[:, sl], wco[:, sl]]
    cols.append(wo)
    wbig = np.ascontiguousarray(np.concatenate(cols, axis=1))
    assert wbig.shape == (1024, WBIG_COLS)
    wr = np.asarray(inp["w_router"], np.float32)[0]
    wr_l = np.ascontiguousarray(wr.reshape(8, 128, 16).transpose(1, 0, 2).reshape(128, 128))
    cwv = np.asarray(inp["conv_w"], np.float32)[0]
    cw_l = np.ascontiguousarray(cwv.reshape(3, 8, 128).transpose(2, 1, 0).reshape(128, 24))
    gbs = [inp["ln0_g"], inp["ln0_b"], inp["ln1_g"][0], inp["ln1_b"][0], inp["ln2_g"][0], inp["ln2_b"][0]]
    gb = np.concatenate([np.broadcast_to(np.asarray(a, np.float32)[None, :], (128, D)) for a in gbs], axis=1)
    sinkb = np.broadcast_to(np.asarray(inp["attn_sink"], np.float32)[0][None, :], (128, 16))
    sh = {
        "wbig": wbig, "wr": wr_l, "cw": cw_l, "gb": np.ascontiguousarray(gb), "sinkb": np.ascontiguousarray(sinkb),
        "emat": _alibi_table(), "ident": np.eye(128, dtype=np.float32), "ones16": np.ones((16, 16), np.float32),
        "offs": (np.arange(64)[:, None] // 16 * SEQ).astype(np.float32),
        "w_gate": np.ascontiguousarray(np.asarray(inp["w_gate"], np.float32)[0]),
        "w_up": np.ascontiguousarray(np.asarray(inp["w_up"], np.float32)[0]),
        "w_down": np.ascontiguousarray(np.asarray(inp["w_down"], np.float32)[0]),
    }
    return sh


_NC_CACHE = {}


def kernel(**inputs):
    x = np.asarray(inputs["x"], np.float32)
    B = x.shape[0]
    nseq = B // NCORES
    sh = _prep_shared(inputs)
    if nseq not in _NC_CACHE:
        _NC_CACHE[nseq] = build(nseq)
    nc = _NC_CACHE[nseq]
    in_maps = []
    for c in range(NCORES):
        m = dict(sh)
        m["x"] = np.ascontiguousarray(x[c * nseq:(c + 1) * nseq].reshape(nseq * SEQ, D))
        in_maps.append(m)
    res = run_bass_kernel_spmd(nc, in_maps, core_ids=list(range(NCORES)))
    out = np.concatenate([r["out"].reshape(nseq, SEQ, D) for r in res.results], axis=0)
    return out.astype(np.float32)
```

```python
import numpy as np
import concourse.bass as bass
import concourse.mybir as mybir
from concourse.bass_utils import run_bass_kernel_spmd

F32 = mybir.dt.float32
BF16 = mybir.dt.bfloat16
I32 = mybir.dt.int32
U32 = mybir.dt.uint32
AF = mybir.ActivationFunctionType
ALU = mybir.AluOpType

D = 1024
SEQ = 2048
NT16 = SEQ // 128
NEXP = 16
CAP = 256
DFF = 2048
ALPHA = 2.0 ** 0.25
EPS = 1e-5
NCORES = 8
WBIG_COLS = 9728
OQ, OKV, OCONV, OE1, OE2, OWO = 0, 1024, 1536, 4608, 6656, 8704


class Op:
    __slots__ = ("eng", "fn", "deps", "odeps", "needed", "sem", "val", "dma", "ndma", "occ", "lat", "idx", "start", "fin")

    def __init__(self, eng, fn, dma=None, ndma=0):
        self.eng = eng
        self.fn = fn
        self.deps = set()
        self.odeps = set()
        self.needed = False
        self.sem = None
        self.val = 0
        self.dma = dma
        self.ndma = ndma


class DSem:
    def __init__(self, name):
        self.name = name
        self.count = 0
        self.h = None
        self.last = None


DEFC = {"pe": 1.0, "act": 0.6, "dve": 0.65, "pool": 3.0, "sp": 0.1}


class Sched:
    ENGS = ("pe", "act", "dve", "pool", "sp")

    def __init__(self):
        self.ops = {e: [] for e in self.ENGS}
        self.all = []
        self.lastw = {}
        self.readers = {}
        self.dsems = []
        self.final_waits = []

    def dsem(self, name):
        d = DSem(name)
        self.dsems.append(d)
        return d

    def add(self, eng, fn, r=(), w=(), dma=None, ndma=1, c=None, lat=None, soft=False):
        op = Op(eng, fn, dma, ndma if dma is not None else 0)
        deps = op.deps
        for k in r:
            x = self.lastw.get(k)
            if x is not None:
                deps.add(x)
        for k in w:
            x = self.lastw.get(k)
            if x is not None:
                deps.add(x)
            rs = self.readers.get(k)
            if rs:
                deps.update(rs)
        for k in w:
            self.lastw[k] = op
            self.readers[k] = []
        for k in r:
            self.readers.setdefault(k, []).append(op)
        deps.discard(op)
        if eng == "pe":
            op.deps = deps = {d for d in deps if not (d.eng == "pe" and d.dma is None)}
        if soft:
            same = {d for d in deps if d.eng == eng and d.dma is None}
            op.odeps |= same
            op.deps = deps = deps - same
        for d in deps:
            d.needed = True
        if dma is not None:
            dma.count += 16 * ndma
            op.sem = dma
            op.val = dma.count
            if dma.last is not None and dma.last not in deps:
                op.odeps.add(dma.last)
            dma.last = op
            op.occ = c if c is not None else (0.12 if eng == "sp" else 1.0)
            op.lat = lat if lat is not None else 3.0
        else:
            op.occ = c if c is not None else DEFC[eng]
            op.lat = op.occ
        op.idx = len(self.all)
        self.all.append(op)
        return op

    def reorder(self):
        import heapq
        succ = {}
        nd = {}
        for op in self.all:
            nd[op] = len(op.deps) + len(op.odeps)
            for d in op.deps:
                succ.setdefault(d, []).append((op, 0))
            for d in op.odeps:
                succ.setdefault(d, []).append((op, 1))
        rt = {}
        avail = {e: [] for e in self.ENGS}
        future = {e: [] for e in self.ENGS}
        T = {e: 0.0 for e in self.ENGS}
        for op in self.all:
            if nd[op] == 0:
                heapq.heappush(avail[op.eng], (op.idx, op))
                rt[op] = 0.0
        order = {e: [] for e in self.ENGS}
        n = len(self.all)
        done = 0
        while done < n:
            best = None
            for e in self.ENGS:
                fu, av = future[e], avail[e]
                while fu and fu[0][0] <= T[e]:
                    _, i, o = heapq.heappop(fu)
                    heapq.heappush(av, (i, o))
                if av:
                    cand = (T[e], av[0][0], e, 0)
                elif fu:
                    cand = (fu[0][0], fu[0][1], e, 1)
                else:
                    continue
                if best is None or cand < best:
                    best = cand
            st, _, e, src = best
            if src == 0:
                _, op = heapq.heappop(avail[e])
            else:
                _, _, op = heapq.heappop(future[e])
            op.start = st
            op.fin = st + op.lat
            T[e] = st + op.occ
            order[e].append(op)
            done += 1
            for (o2, kind) in succ.get(op, ()):
                t = op.start if kind == 1 else op.fin
                if rt.get(o2, 0.0) < t:
                    rt[o2] = t
                nd[o2] -= 1
                if nd[o2] == 0:
                    heapq.heappush(future[o2.eng], (rt.get(o2, 0.0), o2.idx, o2))
        self.ops = order
        self.sim_T = max(T.values())

    def emit(self, nc, block, ctxs):
        engsem = {}
        for e in self.ENGS:
            engsem[e] = ctxs.enter_context(nc.semaphore("prog_" + e))
        for d in self.dsems:
            d.h = ctxs.enter_context(nc.semaphore("d_" + d.name))
        for e in self.ENGS:
            c = 0
            for op in self.ops[e]:
                if op.dma is None and op.needed:
                    c += 1
                    op.sem = e
                    op.val = c

        def handle(s):
            return engsem[s] if isinstance(s, str) else s.h

        def run(ename, eng):
            waited = {}
            for op in self.ops[ename]:
                need = {}
                for d in op.deps:
                    k = d.sem
                    if need.get(k, 0) < d.val:
                        need[k] = d.val
                for k, v in need.items():
                    if waited.get(k, 0) < v:
                        eng.wait_ge(handle(k), v)
                        waited[k] = v
                if op.fn is None:
                    continue
                if op.dma is not None:
                    op.fn(eng, op.dma.h)
                else:
                    ins = op.fn(eng)
                    if op.needed:
                        ins.then_inc(engsem[ename], 1)
            if ename == "sp":
                for d, v in self.final_waits:
                    eng.wait_ge(d.h, v)

        block.tensor(lambda e: run("pe", e))
        block.scalar(lambda e: run("act", e))
        block.vector(lambda e: run("dve", e))
        block.gpsimd(lambda e: run("pool", e))
        block.sync(lambda e: run("sp", e))


def bk(name, lo, hi, bs):
    return [(name, b) for b in range(lo // bs, (hi - 1) // bs + 1)]


CFG = {"lnA": "fused", "lnF": "fused", "nzF": 4, "lnFin": "act", "nfs": 9, "soft_topk": False}


def build(NSEQ, debug=False, cfg=None):
    cfg = dict(CFG, **(cfg or {}))
    from contextlib import ExitStack

    NT = NSEQ * SEQ
    nc = bass.Bass("TRN2", target_bir_lowering=False)
    S = Sched()
    kind_dbg = "ExternalOutput" if debug else "Internal"

    def din(name, shape, dt=F32):
        return nc.dram_tensor(name, shape, dt, kind="ExternalInput").ap()

    x_d = din("x", [NT, D])
    wbig_d = din("wbig", [D, WBIG_COLS])
    wr_d = din("wr", [128, 8 * 16])
    cw_d = din("cw", [128, 24])
    gb_d = din("gb", [128, 6 * D])
    sink_d = din("sinkb", [128, 16])
    emat_d = din("emat", [128, 3 * 16 * 128])
    ident_d = din("ident", [128, 128])
    ones_d = din("ones16", [16, 16])
    offs_d = din("offs", [64, 1])
    wg_d = din("w_gate", [NEXP, D, DFF])
    wu_d = din("w_up", [NEXP, D, DFF])
    wd_d = din("w_down", [NEXP, DFF, D])
    out_d = nc.dram_tensor("out", [NT, D], F32, kind="ExternalOutput").ap()
    xn_d = nc.dram_tensor("xn_s", [NT, D], F32, kind="Internal").ap()
    x1b_d = nc.dram_tensor("x1b_s", [NT, D], BF16, kind=kind_dbg).ap()
    acc_d = nc.dram_tensor("acc_s", [NT, D], F32, kind=kind_dbg).ap()
    aff_d = nc.dram_tensor("aff_s", [64, SEQ], F32, kind=kind_dbg).ap()
    if debug:
        dbg_val = nc.dram_tensor("dbg_val", [128, 128], F32, kind="ExternalOutput").ap()
        dbg_idx = nc.dram_tensor("dbg_idx", [128, 128], I32, kind="ExternalOutput").ap()

    ctx = ExitStack()
    with ctx:
        def sb(name, shape, dt):
            return ctx.enter_context(nc.sbuf_tensor("s_" + name, shape, dt))

        ctx.enter_context(nc.allow_low_precision("bf16 matmul operands, fp32 accumulate"))
        xnT_t = sb("xnT", [128, 16384], BF16)
        bufQ_t = sb("bufQ", [128, 16384], BF16)
        bufY_t = sb("bufY", [128, 16384], BF16)
        kv_t = sb("kv", [128, 4160], BF16)
        kTm_t = sb("kTm", [128, 8192], BF16)
        wsl_t = sb("wsl", [128, 4, 4096], BF16)
        W_t = sb("W", [128, 6144], F32)
        gb01 = sb("gb01", [128, 4, D], F32)
        emat = sb("emat", [128, 3, 16, 128], BF16)
        identb = sb("identb", [128, 128], BF16)
        identf = sb("identf", [128, 128], F32)
        wr_t = sb("wr", [128, 8, 16], F32)
        cw_t = sb("cw", [128, 8, 3], F32)
        esink = sb("esink", [128, 16], F32)
        ones16 = sb("ones16", [16, 16], F32)
        offs_t = sb("offs", [64, 1], F32)
        st_t = sb("stats", [128, 8, 12], F32)
        mv_t = sb("mv", [128, 8, 2], F32)
        rs_t = sb("rstd", [128, 8, 2], F32)
        den_t = sb("den", [128, 2, 32], F32)
        ps_t = ctx.enter_context(nc.psum_tensor("p_ps", [128, 8, 512], F32))

        xnT = xnT_t[:, :].rearrange("p (c t) -> p c t", c=8)
        bufQ = bufQ_t[:, :].rearrange("p (c t) -> p c t", c=8)
        qv = bufQ_t[:, :].rearrange("p (n c t) -> p n c t", n=16, c=8)

        def kQq(n, c0, c1):
            return bk("bufQ", n * 1024 + c0 * 128, n * 1024 + c1 * 128, 512)
        bufY = bufY_t[:, :].rearrange("p (c t) -> p c t", c=8)
        kTm = kTm_t[:, :].rearrange("p (g t) -> p g t", g=4)
        vext = kv_t[:, :].rearrange("p (t g e) -> p t g e", t=16, g=4)

        def kxnT(c0, c1, t0, t1):
            ks = []
            for c in range(c0, c1):
                ks += bk("xnT", c * 2048 + t0, c * 2048 + t1, 512)
            return ks

        def kQ(c0, c1, t0, t1):
            ks = []
            for c in range(c0, c1):
                ks += bk("bufQ", c * 2048 + t0, c * 2048 + t1, 512)
            return ks

        def kY(c0, c1, t0, t1):
            ks = []
            for c in range(c0, c1):
                ks += bk("bufY", c * 2048 + t0, c * 2048 + t1, 512)
            return ks

        def kkT(g, t0, t1):
            return bk("kTm", g * 2048 + t0, g * 2048 + t1, 128)

        def kV(t):
            return bk("kv", t * 260, (t + 1) * 260, 128)

        def Wf(off, n):
            return W_t[:, off:off + n]

        def Wb(off, n):
            return W_t[:, off:off + n].bitcast(BF16)

        def kW(off, n):
            return bk("W", off, off + n, 128)

        def psb(b):
            return ps_t[:, b, :]

        HB = [(0, 7), (7, 14), (14, 16)]

        def kps(b, nb=1):
            ks = []
            for i in range(b, b + nb):
                ks.append(("ps", i))
                if 4 <= i <= 6:
                    ks += [("psO", h) for h in range(*HB[i - 4])]
            return ks

        ring = {"b": 0, "c": 0}

        def nbank():
            b = ring["b"]
            ring["b"] = (b + 1) % 4
            return b

        wsem = [S.dsem("w%d" % i) for i in range(4)]
        pieces = []
        for s in range(NSEQ):
            pieces += [("big", OQ, 512), ("big", OQ + 512, 512), ("big", OKV, 512)]
            pieces += [("big", OE1 + 512 * i, 512) for i in range(4)]
            pieces += [("big", OCONV + 384 * c, 384) for c in range(8)]
            pieces += [("big", OE2 + 512 * i, 512) for i in range(4)]
            pieces += [("big", OWO, 512), ("big", OWO + 512, 512)]
        NP1 = len(pieces)
        for e in range(NEXP):
            for fg in range(4):
                pieces += [("g", e, fg), ("u", e, fg)]
        pstate = {"issued": 0}

        def piece_issue(i):
            p = pieces[i]
            slot = i % 4
            dst = wsl_t[:, slot, :]
            if p[0] == "big":
                _, c0, n = p
                src = wbig_d[:, c0:c0 + n].rearrange("(k p) f -> p k f", p=128)
                d = dst[:, 0:8 * n].rearrange("p (k f) -> p k f", k=8)
            else:
                wsrc = wg_d if p[0] == "g" else wu_d
                src = wsrc[p[1], :, p[2] * 512:(p[2] + 1) * 512].rearrange("(k p) f -> p k f", p=128)
                d = dst.rearrange("p (k f) -> p k f", k=8)
            S.add("pool", lambda e, sem, d=d, src=src: e.dma_start(out=d, in_=src).then_inc(sem, 16),
                  w=[("wsl", slot)], dma=wsem[slot], c=1.5, lat=12.0)

        def want(i, ahead=2):
            tgt = min(len(pieces), i + ahead + 1)
            while pstate["issued"] < tgt:
                piece_issue(pstate["issued"])
                pstate["issued"] += 1

        def pslot(i, n):
            return wsl_t[:, i % 4, 0:8 * n].rearrange("p (k f) -> p k f", k=8)

        csem = S.dsem("const")
        csem2 = S.dsem("constsw")

        cops = []

        def cload(eng, dst, src, key):
            cops.append(S.add(eng, lambda e, sem: e.dma_start(out=dst, in_=src).then_inc(sem, 16), w=[key],
                              dma=(csem if eng == "sp" else csem2)))

        cload("sp", gb01[:, :, :], gb_d[:, 0:4 * D].rearrange("p (a d) -> p a d", a=4), "gb01")
        cload("sp", identf[:, :], ident_d, "identf")
        cload("sp", wr_t[:, :, :], wr_d.rearrange("p (k e) -> p k e", k=8), "wr")
        cload("sp", cw_t[:, :, :], cw_d.rearrange("p (c w) -> p c w", c=8), "cw")
        cload("sp", esink[:, :], sink_d, "esink")
        cload("sp", ones16[:, :], ones_d, "ones16")
        cload("sp", offs_t[:, :], offs_d, "offs")
        cload("pool", identb[:, :], ident_d, "identb")
        cload("pool", emat[:, :, :, :], emat_d.rearrange("p (a h t) -> p a h t", a=3, h=16), "emat")
        for o in cops:
            o.val = o.sem.count
        S.add("act", lambda e: e.activation(out=esink[:, :], in_=esink[:, :], func=AF.Exp), r=["esink"], w=["esink"])
        S.add("pool", lambda e: e.memset(kv_t[:, :], 1.0), w=bk("kv", 0, 4160, 128))
        S.add("pool", lambda e: e.memset(kTm_t[:, :], 0.0), w=bk("kTm", 0, 8192, 128))

        lnc = {"i": 0}

        def layer_norm(src_ap, src_keys, dst_ap, dst_keys, g_ap, b_ap, gbkey, eps=EPS, act_stats=None, style="old"):
            i = lnc["i"] % 8
            lnc["i"] += 1
            st = st_t[:, i, :]
            kst, kmv, krs = ("st", i), ("mv", i), ("rs", i)
            if style != "act":
                S.add("dve", lambda e: e.bn_stats(out=st[:, 0:6], in_=src_ap[:, 0:512]), r=src_keys, w=[(kst, 0)], c=0.65)
                S.add("dve", lambda e: e.bn_stats(out=st[:, 6:12], in_=src_ap[:, 512:1024]), r=src_keys, w=[(kst, 1)], c=0.65)
                S.add("dve", lambda e: e.bn_aggr(out=mv_t[:, i, :], in_=st), r=[(kst, 0), (kst, 1)], w=[kmv], c=0.2)
            else:
                junk, kjunk = act_stats
                S.add("act", lambda e: e.activation(out=src_ap, in_=src_ap, func=AF.Identity, accum_out=st[:, 0:1]),
                      r=src_keys, w=src_keys + [(kst, 8)], c=1.0)
                S.add("act", lambda e: e.activation(out=junk, in_=src_ap, func=AF.Square, accum_out=st[:, 1:2]),
                      r=src_keys, w=kjunk + [(kst, 9)], c=1.0)
                S.add("act", lambda e: e.activation(out=st[:, 4:5], in_=st[:, 5:6], func=AF.Copy),
                      r=[(kst, 8), (kst, 9)], w=[(kst, 0), (kst, 1)], c=0.25)
                S.add("dve", lambda e: e.tensor_scalar(out=mv_t[:, i, 0:1], in0=st[:, 0:1], scalar1=1.0 / D, scalar2=None, op0=ALU.mult),
                      r=[(kst, 0)], w=[(kmv, 0)], c=0.15)
                S.add("dve", lambda e: e.tensor_tensor(out=st[:, 2:3], in0=mv_t[:, i, 0:1], in1=mv_t[:, i, 0:1], op=ALU.mult),
                      r=[(kmv, 0)], w=[(kst, 2)], c=0.15)
                S.add("dve", lambda e: e.scalar_tensor_tensor(out=mv_t[:, i, 1:2], in0=st[:, 1:2], scalar=1.0 / D, in1=st[:, 2:3],
                                                              op0=ALU.mult, op1=ALU.subtract),
                      r=[(kst, 1), (kst, 2), (kmv, 0)], w=[kmv], c=0.15)
            S.add("act", lambda e: e.activation(out=rs_t[:, i, 0:1], in_=mv_t[:, i, 1:2], func=AF.Sqrt, bias=eps, scale=1.0),
                  r=[kmv], w=[(krs, 0)], c=0.3)
            S.add("dve", lambda e: e.reciprocal(out=rs_t[:, i, 0:1], in_=rs_t[:, i, 0:1]), r=[(krs, 0)], w=[(krs, 0)], c=0.15)
            if style == "old":
                S.add("dve", lambda e: e.scalar_tensor_tensor(out=rs_t[:, i, 1:2], in0=mv_t[:, i, 0:1], scalar=-1.0,
                                                              in1=rs_t[:, i, 0:1], op0=ALU.mult, op1=ALU.mult),
                      r=[kmv, (kmv, 0), (krs, 0)], w=[(krs, 1)], c=0.12)
                S.add("act", lambda e: e.activation(out=dst_ap, in_=src_ap, func=AF.Identity, scale=rs_t[:, i, 0:1],
                                                    bias=rs_t[:, i, 1:2]), r=src_keys + [(krs, 0), (krs, 1)], w=dst_keys, c=1.0)
                S.add("dve", lambda e: e.tensor_tensor(out=dst_ap, in0=dst_ap, in1=g_ap, op=ALU.mult),
                      r=dst_keys + [gbkey], w=dst_keys, c=1.2)
                S.add("dve", lambda e: e.tensor_tensor(out=dst_ap, in0=dst_ap, in1=b_ap, op=ALU.add),
                      r=dst_keys + [gbkey], w=dst_keys, c=1.2)
            else:
                S.add("dve", lambda e: e.scalar_tensor_tensor(out=dst_ap, in0=src_ap, scalar=mv_t[:, i, 0:1], in1=g_ap,
                                                              op0=ALU.subtract, op1=ALU.mult),
                      r=src_keys + [kmv, (kmv, 0), gbkey], w=dst_keys, c=1.2)
                S.add("dve", lambda e: e.scalar_tensor_tensor(out=dst_ap, in0=dst_ap, scalar=rs_t[:, i, 0:1], in1=b_ap,
                                                              op0=ALU.mult, op1=ALU.add),
                      r=dst_keys + [(krs, 0), gbkey], w=dst_keys, c=1.2)

        def mm_group(out_ap, pairs, rkeys, wkeys, c=None):
            n = len(pairs)
            if c is None:
                c = 0.08 + n * 0.256

            def fn(e):
                ins = None
                for i, (l, r) in enumerate(pairs):
                    ins = e.matmul(out_ap, l, r, start=(i == 0), stop=(i == n - 1))
                return ins
            S.add("pe", fn, r=rkeys, w=wkeys, c=c)

        def transposes8(src_ap, src_keys, bank, ident, nbk=1, dt=BF16):
            if dt == BF16:
                dst = ps_t[:, bank, :].bitcast(BF16).rearrange("p (c t) -> p c t", c=8)
            else:
                dst = ps_t[:, bank:bank + 2, :].rearrange("p a (c t) -> p (a c) t", c=4)

            def fn(e):
                ins = None
                for c in range(8):
                    ins = e.transpose(dst[:, c, :], src_ap[:, c * 128:(c + 1) * 128], ident)
                return ins
            S.add("pe", fn, r=src_keys, w=kps(bank, nbk), c=(0.7 if dt == BF16 else 2.2))
            return dst

        xin_sem = [S.dsem("xin%d" % i) for i in range(4)]
        xns_sem = [S.dsem("xns%d" % i) for i in range(4)]
        xnr_sem = [S.dsem("xnr%d" % i) for i in range(4)]
        x1s_sem = [S.dsem("x1s%d" % i) for i in range(2)]
        acs_sem = [S.dsem("acs%d" % i) for i in range(4)]
        afs_sem = S.dsem("afs")

        pi = 0
        for s in range(NSEQ):
            T0 = s * SEQ
            want(pi, 2)
            for t in range(NT16):
                sl = t % 4
                r0 = T0 + t * 128
                if sl < 2:
                    xin = Wf(sl * 1024, 1024)
                    kxin = kW(sl * 1024, 1024)
                else:
                    xo = 10240 + (sl - 2) * 2048
                    xin = bufY_t[:, xo:xo + 2048].bitcast(F32)
                    kxin = bk("bufY", xo, xo + 2048, 512)
                xn16 = bufY_t[:, (t % 2) * 1024:(t % 2 + 1) * 1024]
                kxn16 = bk("bufY", (t % 2) * 1024, (t % 2 + 1) * 1024, 512)
                S.add("sp", lambda e, sem, xin=xin, r0=r0: e.dma_start(out=xin, in_=x_d[r0:r0 + 128, :]).then_inc(sem, 16),
                      w=kxin, dma=xin_sem[sl], lat=4.0)
                layer_norm(xin, kxin, xin, kxin, gb01[:, 0, :], gb01[:, 1, :], "gb01", style=cfg["lnA"])
                S.add("act", lambda e, a=xn16, b=xin: e.activation(out=a, in_=b, func=AF.Copy), r=kxin, w=kxn16, c=1.0)
                S.add("sp", lambda e, sem, a=xin, r0=r0: e.dma_start(out=xn_d[r0:r0 + 128, :], in_=a).then_inc(sem, 16),
                      r=kxin, w=[("xn_d", s, t)], dma=xns_sem[sl], lat=4.0)
                pt = transposes8(xn16, kxn16 + ["identb"], 7, identb[:, :])
                S.add("act", lambda e, pt=pt, t=t: e.activation(out=xnT[:, :, t * 128:(t + 1) * 128], in_=pt, func=AF.Copy),
                      r=kps(7), w=kxnT(0, 8, t * 128, (t + 1) * 128), c=1.0)

            for qp in range(2):
                want(pi, 2)
                wv = pslot(pi, 512)
                for cc in range(4):
                    c = qp * 4 + cc
                    for tg in range(4):
                        b = nbank()
                        mm_group(psb(b), [(wv[:, k, cc * 128:(cc + 1) * 128], xnT[:, k, tg * 512:(tg + 1) * 512]) for k in range(8)],
                                 [("wsl", pi % 4)] + kxnT(0, 8, tg * 512, (tg + 1) * 512), kps(b))
                        S.add("act", lambda e, b=b, c=c, tg=tg: e.activation(
                            out=qv[:, tg * 4:(tg + 1) * 4, c, :], in_=psb(b).rearrange("p (n t) -> p n t", n=4),
                            func=AF.Identity, scale=0.125),
                            r=kps(b), w=sum([kQq(tg * 4 + i, c, c + 1) for i in range(4)], []), c=0.65)
                pi += 1
            want(pi, 2)
            wv = pslot(pi, 512)
            for j in range(2):
                for tg in range(4):
                    b = nbank()
                    mm_group(psb(b), [(wv[:, k, j * 128:(j + 1) * 128], xnT[:, k, tg * 512:(tg + 1) * 512]) for k in range(8)],
                             [("wsl", pi % 4)] + kxnT(0, 8, tg * 512, (tg + 1) * 512), kps(b))
                    S.add("dve", lambda e, b=b, j=j, tg=tg: e.tensor_copy(kTm[0:64, j, tg * 512:(tg + 1) * 512], ps_t[0:64, b, :]),
                          r=kps(b), w=kkT(j, tg * 512, (tg + 1) * 512), c=0.65)
                    S.add("dve", lambda e, b=b, j=j, tg=tg: e.tensor_copy(kTm[64:128, j + 2, tg * 512:(tg + 1) * 512],
                                                                          ps_t[64:128, b, :]),
                          r=kps(b), w=kkT(j + 2, tg * 512, (tg + 1) * 512), c=0.65)
            for t in range(NT16):
                b = nbank()
                mm_group(ps_t[:, b, 0:256], [(xnT[:, k, t * 128:(t + 1) * 128], wv[:, k, 256:512]) for k in range(8)],
                         [("wsl", pi % 4)] + kxnT(0, 8, t * 128, (t + 1) * 128), kps(b))
                S.add("dve", lambda e, b=b, t=t: e.tensor_copy(vext[:, t, :, 0:64],
                                                             ps_t[:, b, 0:256].rearrange("p (g e) -> p g e", g=4)),
                      r=kps(b), w=kV(t))
            pi += 1

            want(pi, 2)
            def o_ap(h):
                bnk = 4 + h // 7
                o = (h % 7) * 65
                return ps_t[:, bnk, o:o + 65]

            for n in range(NT16):
                kbs = [kb for kb in (n - 1, n, n + 1) if 0 <= kb < NT16]
                for g in range(4):
                    r0 = 64 * (g // 2)
                    kc = g % 2
                    c0 = (g % 2) * 4
                    pts = []
                    for kbi, kb in enumerate(kbs):
                        typ = kb - n + 1
                        b = nbank()
                        eb = Wb(kbi * 256, 256)
                        keb = kW(kbi * 256, 256)
                        pslot_i = (g % 2) * 3 + kbi
                        pT = Wb(1536 + pslot_i * 256, 256)
                        kpT = kW(1536 + pslot_i * 256, 256)
                        pts.append((pT, kpT))
                        mm_group(psb(b).rearrange("p (h t) -> p h t", h=4), [(kTm[:, g, kb * 128:(kb + 1) * 128],
                                           qv[:, n, c0:c0 + 4, :])],
                                 kkT(g, kb * 128, (kb + 1) * 128) + kQq(n, c0, c0 + 4), kps(b), c=0.3)
                        S.add("act", lambda e, eb=eb, b=b: e.activation(out=eb, in_=psb(b), func=AF.Exp), r=kps(b), w=keb)
                        S.add("dve", lambda e, pT=pT, eb=eb, typ=typ, g=g: e.tensor_tensor(
                            out=pT.rearrange("p (h t) -> p h t", h=4), in0=eb.rearrange("p (h t) -> p h t", h=4),
                            in1=emat[:, typ, 4 * g:4 * g + 4, :], op=ALU.mult), r=keb + ["emat"], w=kpT)
                    for r in range(4):
                        h = 4 * g + r
                        mm_group(o_ap(h), [(pts[kbi][0][:, r * 128:(r + 1) * 128], vext[:, kb, g, :]) for kbi, kb in enumerate(kbs)],
                                 sum([pts[kbi][1] for kbi in range(len(kbs))], []) + sum([kV(kb) for kb in kbs], []),
                                 [("psO", h)], c=0.25)
                di = n % 2
                den = den_t[:, di, 0:16]
                rden = den_t[:, di, 16:32]
                yat = Wb(3072 + di * 512, 512)
                kyat = kW(3072 + di * 512, 512)
                for bi, (h0, h1) in enumerate(HB):
                    nh = h1 - h0
                    ov = ps_t[:, 4 + bi, 0:nh * 65].rearrange("p (h e) -> p h e", h=nh)
                    S.add("dve", lambda e, ov=ov, h0=h0, h1=h1, den=den: e.tensor_tensor(
                        out=den[:, h0:h1], in0=ov[:, :, 64], in1=esink[:, h0:h1], op=ALU.add),
                        r=[("psO", h) for h in range(h0, h1)] + ["esink"], w=[("den", di, bi)], c=0.15)
                S.add("dve", lambda e, den=den, rden=rden: e.reciprocal(out=rden, in_=den),
                      r=[("den", di, bi) for bi in range(3)], w=[("rden", di)])
                for bi, (h0, h1) in enumerate(HB):
                    nh = h1 - h0
                    ov = ps_t[:, 4 + bi, 0:nh * 65].rearrange("p (h e) -> p h e", h=nh)
                    S.add("dve", lambda e, ov=ov, h0=h0, h1=h1, nh=nh, rden=rden, yat=yat: e.tensor_tensor(
                        out=yat[:, h0 * 64:h1 * 64].rearrange("p (h e) -> p h e", h=nh), in0=ov[:, :, 0:64],
                        in1=rden[:, h0:h1].unsqueeze(2).to_broadcast([128, nh, 64]), op=ALU.mult),
                        r=[("psO", h) for h in range(h0, h1)] + [("rden", di)], w=kyat, c=0.6)
                pt = transposes8(yat, kyat + ["identb"], 7, identb[:, :])
                S.add("act", lambda e, pt=pt, n=n: e.activation(out=bufY[:, :, n * 128:(n + 1) * 128], in_=pt, func=AF.Copy),
                      r=kps(7), w=kY(0, 8, n * 128, (n + 1) * 128))

            for ep in range(4):
                want(pi, 2)
                wv = pslot(pi, 512)
                for cc in range(2):
                    c = ep * 2 + cc
                    for tg in range(4):
                        b1 = nbank()
                        b2 = nbank()
                        tsl = slice(tg * 512, (tg + 1) * 512)
                        mm_group(psb(b1), [(wv[:, k, cc * 256:cc * 256 + 128], xnT[:, k, tsl]) for k in range(8)],
                                 [("wsl", pi % 4)] + kxnT(0, 8, tg * 512, (tg + 1) * 512), kps(b1))
                        mm_group(psb(b2), [(wv[:, k, cc * 256 + 128:cc * 256 + 256], bufY[:, k, tsl]) for k in range(8)],
                                 [("wsl", pi % 4)] + kY(0, 8, tg * 512, (tg + 1) * 512), kps(b2))
                        si = (c * 4 + tg) % 2
                        sg = Wf(si * 512, 512)
                        ksg = kW(si * 512, 512)
                        S.add("act", lambda e, sg=sg, b1=b1: e.activation(out=sg, in_=psb(b1), func=AF.Sigmoid), r=kps(b1), w=ksg)
                        S.add("dve", lambda e, sg=sg, b2=b2, c=c, tsl=tsl: e.tensor_tensor(out=bufQ[:, c, tsl], in0=psb(b2), in1=sg,
                                                                                         op=ALU.mult),
                              r=kps(b2) + ksg, w=kQ(c, c + 1, tg * 512, (tg + 1) * 512))
                pi += 1

            UO = 1024
            for c in range(8):
                want(pi, 2)
                wv = pslot(pi, 384)
                u = Wf(UO, 2050)
                S.add("dve", lambda e, u=u: e.memset(u[:, 0:1], 0.0), w=kW(UO, 1), c=0.1)
                S.add("dve", lambda e, u=u: e.memset(u[:, 2049:2050], 0.0), w=kW(UO + 2049, 1), c=0.1)
                cbank = {}

                def conv_out(tgi, c=c, u=u, cbank=cbank):
                    j0 = tgi * 512
                    yi = tgi % 2
                    yt = Wf(3200 + yi * 512, 512)
                    kyt = kW(3200 + yi * 512, 512)
                    bC = cbank[tgi]
                    ku = kW(UO + j0, 514)
                    S.add("act", lambda e: e.activation(out=yt, in_=u[:, j0:j0 + 512], func=AF.Identity, scale=cw_t[:, c, 0:1]),
                          r=ku + ["cw"], w=kyt, c=0.65)
                    S.add("dve", lambda e: e.scalar_tensor_tensor(out=yt, in0=u[:, j0 + 1:j0 + 513], scalar=cw_t[:, c, 1:2], in1=yt,
                                                                  op0=ALU.mult, op1=ALU.add), r=ku + ["cw"] + kyt, w=kyt, c=0.65)
                    S.add("dve", lambda e: e.scalar_tensor_tensor(out=yt, in0=u[:, j0 + 2:j0 + 514], scalar=cw_t[:, c, 2:3], in1=yt,
                                                                  op0=ALU.mult, op1=ALU.add), r=ku + ["cw"] + kyt, w=kyt, c=0.65)
                    S.add("dve", lambda e: e.tensor_tensor(out=bufY[:, c, j0:j0 + 512], in0=yt, in1=psb(bC), op=ALU.mult),
                          r=kyt + kps(bC), w=kY(c, c + 1, j0, j0 + 512), c=0.65)

                for tg in range(4):
                    tsl = slice(tg * 512, (tg + 1) * 512)
                    bA, bB = nbank(), nbank()
                    bC = 4 + (ring["c"] % 3)
                    ring["c"] += 1
                    cbank[tg] = bC
                    rk = [("wsl", pi % 4)] + kxnT(0, 8, tg * 512, (tg + 1) * 512)
                    mm_group(psb(bA), [(wv[:, k, 0:128], xnT[:, k, tsl]) for k in range(8)], rk, kps(bA))
                    mm_group(psb(bB), [(wv[:, k, 128:256], xnT[:, k, tsl]) for k in range(8)], rk, kps(bB))
                    mm_group(psb(bC), [(wv[:, k, 256:384], xnT[:, k, tsl]) for k in range(8)], rk, kps(bC))
                    ci = tg % 2
                    ccs = Wf(ci * 512, 512)
                    kccs = kW(ci * 512, 512)
                    S.add("act", lambda e, ccs=ccs, bA=bA: e.activation(out=ccs, in_=psb(bA), func=AF.Copy), r=kps(bA), w=kccs, c=0.65)
                    S.add("dve", lambda e, ccs=ccs, bB=bB, tg=tg, u=u: e.tensor_tensor(out=u[:, 1 + tg * 512:1 + (tg + 1) * 512],
                                                                                     in0=psb(bB), in1=ccs, op=ALU.mult),
                          r=kps(bB) + kccs, w=kW(UO + 1 + tg * 512, 512), c=0.65)
                    if tg >= 1:
                        conv_out(tg - 1)
                conv_out(3)
                pi += 1

            for ep in range(4):
                want(pi, 2)
                wv = pslot(pi, 512)
                for cc in range(2):
                    c = ep * 2 + cc
                    for tg in range(4):
                        b1 = nbank()
                        b2 = nbank()
                        tsl = slice(tg * 512, (tg + 1) * 512)
                        mm_group(psb(b1), [(wv[:, k, cc * 256:cc * 256 + 128], xnT[:, k, tsl]) for k in range(8)],
                                 [("wsl", pi % 4)] + kxnT(0, 8, tg * 512, (tg + 1) * 512), kps(b1))
                        mm_group(psb(b2), [(wv[:, k, cc * 256 + 128:cc * 256 + 256], bufY[:, k, tsl]) for k in range(8)],
                                 [("wsl", pi % 4)] + kY(0, 8, tg * 512, (tg + 1) * 512), kps(b2))
                        si = (c * 4 + tg) % 2
                        sg = Wf(si * 512, 512)
                        ksg = kW(si * 512, 512)
                        t2 = Wf(1024 + si * 512, 512)
                        kt2 = kW(1024 + si * 512, 512)
                        S.add("act", lambda e, sg=sg, b1=b1: e.activation(out=sg, in_=psb(b1), func=AF.Sigmoid), r=kps(b1), w=ksg, c=0.65)
                        S.add("dve", lambda e, sg=sg, b2=b2, t2=t2: e.tensor_tensor(out=t2, in0=psb(b2), in1=sg, op=ALU.mult),
                              r=kps(b2) + ksg, w=kt2, c=0.65)
                        S.add("dve", lambda e, t2=t2, c=c, tsl=tsl: e.tensor_tensor(out=bufQ[:, c, tsl], in0=t2, in1=bufQ[:, c, tsl],
                                                                                  op=ALU.add),
                              r=kt2 + kQ(c, c + 1, tg * 512, (tg + 1) * 512), w=kQ(c, c + 1, tg * 512, (tg + 1) * 512), c=0.65)
                pi += 1

            want(pi, 2)
            want(pi + 1, 2)
            wA = pslot(pi, 512)
            wB = pslot(pi + 1, 512)
            kwAB = [("wsl", pi % 4), ("wsl", (pi + 1) % 4)]
            for t in range(NT16):
                r0 = T0 + t * 128
                zi = t % cfg["nzF"]
                zb = Wf(2048 + zi * 1024, 1024)
                kzb = kW(2048 + zi * 1024, 1024)
                xi = t % 2
                x1b = bufY_t[:, 2048 + xi * 1024:2048 + (xi + 1) * 1024]
                kx1b = bk("bufY", 2048 + xi * 1024, 2048 + (xi + 1) * 1024, 512)
                x1T = bufY_t[:, 4096 + xi * 2048:4096 + (xi + 1) * 2048].bitcast(F32).rearrange("p (c t) -> p c t", c=8)
                kx1T = bk("bufY", 4096 + xi * 2048, 4096 + (xi + 1) * 2048, 512)
                ex = bufY_t[0:16, 8192:8448].bitcast(F32)
                rsm = bufY_t[0:16, 8704:8960].bitcast(F32)
                affo = bufY_t[0:16, 9216:9472].bitcast(F32)
                kex, krsm, kaffo = [("bufY", 16)], [("bufY", 17)], [("bufY", 18)]
                S.add("sp", lambda e, sem, zb=zb, r0=r0: e.dma_start(out=zb, in_=xn_d[r0:r0 + 128, :]).then_inc(sem, 16),
                      r=[("xn_d", s, t)], w=kzb, dma=xnr_sem[zi], lat=4.0)
                tok = slice(t * 128, (t + 1) * 128)
                hb = 4 if t % 2 == 0 else 6
                mm_group(psb(hb), [(bufQ[:, k, tok], wA[:, k, :]) for k in range(8)], kwAB + kQ(0, 8, t * 128, (t + 1) * 128), kps(hb))
                mm_group(psb(hb + 1), [(bufQ[:, k, tok], wB[:, k, :]) for k in range(8)], kwAB + kQ(0, 8, t * 128, (t + 1) * 128),
                         kps(hb + 1))
                S.add("dve", lambda e, zb=zb, hb=hb: e.scalar_tensor_tensor(
                    out=zb.rearrange("p (a f) -> p a f", a=2), in0=zb.rearrange("p (a f) -> p a f", a=2), scalar=ALPHA,
                    in1=ps_t[:, hb:hb + 2, :], op0=ALU.mult, op1=ALU.add), r=kzb + kps(hb, 2), w=kzb, c=1.2)
                layer_norm(zb, kzb, zb, kzb, gb01[:, 2, :], gb01[:, 3, :], "gb01", act_stats=(x1b, kx1b), style=cfg["lnF"])
                S.add("act", lambda e, x1b=x1b, zb=zb: e.activation(out=x1b, in_=zb, func=AF.Copy), r=kzb, w=kx1b, c=1.0)
                S.add("sp", lambda e, sem, x1b=x1b, r0=r0: e.dma_start(out=x1b_d[r0:r0 + 128, :], in_=x1b).then_inc(sem, 16),
                      r=kx1b, w=[("x1b_d", s, t)], dma=x1s_sem[xi], lat=3.0)
                S.add("sp", lambda e, sem, zb=zb, r0=r0: e.dma_start(out=acc_d[r0:r0 + 128, :], in_=zb).then_inc(sem, 16),
                      r=kzb, w=[("acc_d", s, t)], dma=acs_sem[zi], lat=4.0)
                tb = 2 if t % 2 == 0 else 0
                pt = transposes8(zb, kzb + ["identf"], tb, identf[:, :], nbk=2, dt=F32)
                S.add("act", lambda e, pt=pt, x1T=x1T: e.activation(out=x1T, in_=pt, func=AF.Copy), r=kps(tb, 2), w=kx1T, c=1.0)
                mm_group(ps_t[0:16, tb, 0:128], [(wr_t[:, k, :], x1T[:, k, :]) for k in range(8)], kx1T + ["wr"], kps(tb), c=0.6)
                S.add("act", lambda e, ex=ex, tb=tb: e.activation(out=ex, in_=ps_t[0:16, tb, 0:128], func=AF.Exp), r=kps(tb), w=kex,
                      c=0.3)
                mm_group(ps_t[0:16, tb + 1, 0:128], [(ones16[:, :], ex)], kex + ["ones16"], kps(tb + 1), c=0.2)
                S.add("dve", lambda e, rsm=rsm, tb=tb: e.reciprocal(out=rsm, in_=ps_t[0:16, tb + 1, 0:128]), r=kps(tb + 1), w=krsm,
                      c=0.25)
                S.add("dve", lambda e, affo=affo, ex=ex, rsm=rsm: e.tensor_tensor(out=affo, in0=ex, in1=rsm, op=ALU.mult),
                      r=kex + krsm, w=kaffo, c=0.25)
                S.add("sp", lambda e, sem, affo=affo, t=t, s=s: e.dma_start(out=aff_d[s * 16:(s + 1) * 16, t * 128:(t + 1) * 128],
                                                                           in_=affo).then_inc(sem, 16),
                      r=kaffo, w=[("aff_d",)], dma=afs_sem, lat=3.0)
            pi += 2
        assert pi == NP1

        NR = NSEQ * 16
        aff_all = W_t[0:NR, 0:2048]
        work = W_t[0:NR, 2048:4096]
        vals = W_t[0:NR, 4096:4352]
        idxu = W_t[0:NR, 4352:4608].bitcast(U32)
        idxf = W_t[0:NR, 4608:4864]
        valT = W_t[:, 4864:4992].rearrange("p (a c) -> p a c", a=2)
        idxT = W_t[:, 4992:5120].bitcast(I32).rearrange("p (a c) -> p a c", a=2)
        tk_sem = S.dsem("tk")
        S.add("sp", lambda e, sem: e.dma_start(out=aff_all, in_=aff_d[0:NR, :]).then_inc(sem, 16),
              r=[("aff_d",)], w=kW(0, 2048) + kW(4096, 1024), dma=tk_sem)
        for it in range(CAP // 8):
            src = aff_all if it == 0 else work
            ksrc = kW(0, 2048) if it == 0 else kW(2048, 2048)
            v8 = vals[:, it * 8:(it + 1) * 8]
            S.add("dve", lambda e, v8=v8, src=src: e.max(out=v8, in_=src), r=ksrc, w=[("v8", it)], c=2.3,
                  soft=(cfg["soft_topk"] and it > 0))
            S.add("dve", lambda e, v8=v8, src=src, it=it: e.max_index(out=idxu[:, it * 8:(it + 1) * 8], in_max=v8, in_values=src),
                  r=ksrc + [("v8", it)], w=[("i8", it)], c=2.3)
            if it < CAP // 8 - 1:
                S.add("dve", lambda e, v8=v8, src=src: e.match_replace(out=work, in_to_replace=v8, in_values=src, imm_value=-1.0),
                      r=ksrc + [("v8", it)], w=kW(2048, 2048), c=2.3, soft=cfg["soft_topk"])
        allv = [("v8", it) for it in range(CAP // 8)]
        alli = [("i8", it) for it in range(CAP // 8)]
        S.add("dve", lambda e: e.tensor_copy(idxf, idxu), r=alli, w=[("idxf",)])
        S.add("dve", lambda e: e.tensor_scalar(out=idxf, in0=idxf, scalar1=offs_t[0:NR, 0:1], scalar2=None, op0=ALU.add),
              r=[("idxf",), "offs"], w=[("idxf",)])

        def tkT(e):
            ins = None
            for half in range(2):
                ins = e.transpose(ps_t[:, 0, half * 64:half * 64 + NR], vals[:, half * 128:(half + 1) * 128], identf[0:NR, 0:NR])
                ins = e.transpose(ps_t[:, 1, half * 64:half * 64 + NR], idxf[:, half * 128:(half + 1) * 128], identf[0:NR, 0:NR])
            return ins
        S.add("pe", tkT, r=allv + [("idxf",), "identf"], w=kps(0, 2))
        S.add("act", lambda e: e.activation(out=valT[:, :, 0:NR], in_=ps_t[:, 0, 0:128].rearrange("p (a c) -> p a c", a=2)[:, :, 0:NR],
                                            func=AF.Identity, scale=1.0 / ALPHA), r=kps(0), w=kW(4864, 128))
        S.add("dve", lambda e: e.tensor_copy(idxT[:, :, 0:NR], ps_t[:, 1, 0:128].rearrange("p (a c) -> p a c", a=2)[:, :, 0:NR]),
              r=kps(1), w=kW(4992, 128))
        if debug:
            dsm = S.dsem("dbg")
            S.add("sp", lambda e, sem: e.dma_start(out=dbg_val, in_=W_t[:, 4864:4992]).then_inc(sem, 16), r=kW(4864, 128), w=[("dbgv",)], dma=dsm)
            S.add("sp", lambda e, sem: e.dma_start(out=dbg_idx, in_=W_t[:, 4992:5120].bitcast(I32)).then_inc(sem, 16), r=kW(4992, 128),
                  w=[("dbgi",)], dma=dsm)
            S.final_waits.append((dsm, 32))

        xg = xnT_t[:, :].rearrange("p (a j d) -> p a j d", a=2, j=8)
        xgT = kTm_t[:, :].rearrange("p (c t) -> p c t", c=8)
        hT = bufQ_t[:, :].rearrange("p (c t) -> p c t", c=16)
        wdv = bufY_t[:, :].rearrange("p (c n) -> p c n", c=16)
        g_sem = [S.dsem("gath%d" % i) for i in range(2)]
        wd_sem = S.dsem("wd")
        sc_sem = [S.dsem("scat%d" % i) for i in range(2)]
        allx1b = [("x1b_d", s, t) for s in range(NSEQ) for t in range(NT16)]

        def kxg(a, j0, j1):
            return bk("xnT", a * 8192 + j0 * 1024, a * 8192 + j1 * 1024, 512)

        def kxgT(j0, j1):
            return [("kTm", c * 8 + jj) for c in range(8) for jj in range(j0, j1)]

        def gather(ex_):
            a = ex_ % 2

            def fn(e, sem):
                for s in range(NSEQ):
                    for half in range(2):
                        col = s * 16 + ex_
                        e.indirect_dma_start(out=xg[:, a, s * 2 + half, :], out_offset=None, in_=x1b_d[:, :],
                                             in_offset=bass.IndirectOffsetOnAxis(ap=idxT[:, half, col:col + 1], axis=0)
                                             ).then_inc(sem, 16)
            S.add("pool", fn, r=allx1b + kW(4992, 128), w=kxg(a, 0, 2 * NSEQ), dma=g_sem[a], ndma=2 * NSEQ, c=2.0 * 2 * NSEQ, lat=2.0 * 2 * NSEQ + 10)

        NJ = 2 * NSEQ
        NSL = NJ * 128
        SH = [(o, min(512, NSL - o)) for o in range(0, NSL, 512)]
        gather(0)
        for ex_ in range(NEXP):
            a = ex_ % 2
            if ex_ + 1 < NEXP:
                gather(ex_ + 1)
            for j in range(NJ):
                pt = transposes8(xg[:, a, j, :], kxg(a, j, j + 1) + ["identb"], 7, identb[:, :])
                eng = "act" if j % 2 == 0 else "dve"
                if eng == "act":
                    S.add("act", lambda e, pt=pt, j=j: e.activation(out=xgT[:, :, j * 128:(j + 1) * 128], in_=pt, func=AF.Copy),
                          r=kps(7), w=kxgT(j, j + 1), c=1.0)
                else:
                    S.add("dve", lambda e, pt=pt, j=j: e.tensor_copy(xgT[:, :, j * 128:(j + 1) * 128], pt),
                          r=kps(7), w=kxgT(j, j + 1), c=1.2)
            for fg in range(4):
                want(pi, 2)
                want(pi + 1, 2)
                wG = pslot(pi, 512)
                wU = pslot(pi + 1, 512)
                for fc in range(4):
                    f = fg * 4 + fc
                    for (so, sn) in SH:
                        bG, bU = nbank(), nbank()
                        rk = kxgT(so // 128, (so + sn) // 128)
                        mm_group(ps_t[:, bG, 0:sn], [(wG[:, k, fc * 128:(fc + 1) * 128], xgT[:, k, so:so + sn]) for k in range(8)],
                                 [("wsl", pi % 4)] + rk, kps(bG))
                        mm_group(ps_t[:, bU, 0:sn], [(wU[:, k, fc * 128:(fc + 1) * 128], xgT[:, k, so:so + sn]) for k in range(8)],
                                 [("wsl", (pi + 1) % 4)] + rk, kps(bU))
                        si = (f + so // 512) % 2
                        sgt = Wf(si * 512, 512)
                        ksgt = kW(si * 512, 512)
                        S.add("act", lambda e, sgt=sgt, bG=bG, sn=sn: e.activation(out=sgt[:, 0:sn], in_=ps_t[:, bG, 0:sn], func=AF.Silu),
                              r=kps(bG), w=ksgt, c=0.65)
                        S.add("dve", lambda e, sgt=sgt, bU=bU, sn=sn, f=f, so=so: e.tensor_tensor(
                            out=hT[:, f, so:so + sn], in0=ps_t[:, bU, 0:sn], in1=sgt[:, 0:sn], op=ALU.mult),
                            r=kps(bU) + ksgt, w=bk("bufQ", f * 1024 + so, f * 1024 + so + sn, 512), c=0.65)
                pi += 2
            def wdl(e, sem, ex_=ex_):
                for q in range(4):
                    e.dma_start(out=wdv[:, 4 * q:4 * q + 4, :],
                                in_=wd_d[ex_, q * 512:(q + 1) * 512, :].rearrange("(c p) n -> p c n", p=128)).then_inc(sem, 16)
            S.add("pool", wdl, w=bk("bufY", 0, 16384, 512), dma=wd_sem, ndma=4, c=4.0, lat=30.0)
            for j in range(NJ):
                s, half = j // 2, j % 2
                col = s * 16 + ex_
                pb = 4 if j % 2 == 0 else 2
                for dh in range(2):
                    mm_group(psb(pb + dh), [(hT[:, f, j * 128:(j + 1) * 128], wdv[:, f, dh * 512:(dh + 1) * 512]) for f in range(16)],
                             [("bufQ", f * 2 + j // 4) for f in range(16)] + [("bufY", f * 2 + dh) for f in range(16)], kps(pb + dh))
                yi = j % 2
                ye = Wf(1024 + yi * 1024, 1024)
                kye = kW(1024 + yi * 1024, 1024)
                S.add("act", lambda e, ye=ye, pb=pb, half=half, col=col: e.activation(
                    out=ye.rearrange("p (a f) -> p a f", a=2), in_=ps_t[:, pb:pb + 2, :], func=AF.Identity,
                    scale=valT[:, half, col:col + 1]), r=kps(pb, 2) + kW(4864, 128), w=kye, c=1.0)
                S.add("pool", lambda e, sem, ye=ye, half=half, col=col: e.indirect_dma_start(
                    out=acc_d[:, :], out_offset=bass.IndirectOffsetOnAxis(ap=idxT[:, half, col:col + 1], axis=0),
                    in_=ye, in_offset=None, compute_op=ALU.add).then_inc(sem, 16),
                    r=kye + kW(4992, 128), w=[("acc_d", s, t) for t in range(NT16)], dma=sc_sem[yi], c=2.0, lat=8.0)

        gb2 = gb01[:, 0:2, :]
        g2_sem = S.dsem("gb2")
        S.add("sp", lambda e, sem: e.dma_start(out=gb2, in_=gb_d[:, 4 * D:6 * D].rearrange("p (a d) -> p a d", a=2)).then_inc(sem, 16),
              w=["gb01"], dma=g2_sem)
        NFS = cfg["nfs"]
        fl_sem = [S.dsem("fl%d" % i) for i in range(NFS)]
        fs_sem = [S.dsem("fs%d" % i) for i in range(NFS)]
        jk = Wb(5120, 512)
        kjk = kW(5120, 512)
        fi = 0
        for s in range(NSEQ):
            for t in range(NT16):
                r0 = s * SEQ + t * 128
                i = fi % NFS
                fi += 1
                if i < 5:
                    fin = Wf(i * 1024, 1024)
                    kfin = kW(i * 1024, 1024)
                else:
                    fo = (i - 5) * 2048
                    fin = xnT_t[:, fo:fo + 2048].bitcast(F32)
                    kfin = bk("xnT", fo, fo + 2048, 512)
                S.add("sp", lambda e, sem, fin=fin, r0=r0: e.dma_start(out=fin, in_=acc_d[r0:r0 + 128, :]).then_inc(sem, 16),
                      r=[("acc_d", s, t)], w=kfin, dma=fl_sem[i], lat=4.0)
                layer_norm(fin, kfin, fin, kfin, gb2[:, 0, :], gb2[:, 1, :], "gb01", eps=EPS / (ALPHA * ALPHA),
                           act_stats=(jk, kjk), style=cfg["lnFin"])
                S.add("sp", lambda e, sem, fin=fin, r0=r0: e.dma_start(out=out_d[r0:r0 + 128, :], in_=fin).then_inc(sem, 16),
                      r=kfin, w=[("out_d", s, t)], dma=fs_sem[i], lat=4.0)
        for d in fs_sem + acs_sem + sc_sem + x1s_sem + [afs_sem]:
            S.final_waits.append((d, d.count))

        S.reorder()
        block = ctx.enter_context(nc.Block())
        S.emit(nc, block, ctx)
    return nc


def _alibi_table():
    slopes = (2.0 ** (-8.0 * np.arange(1, 17) / 16)).astype(np.float64)
    j = np.arange(128)[:, None]
    i = np.arange(128)[None, :]
    E = np.zeros((128, 3, 16, 128), np.float32)
    for typ, dist in enumerate((128 + i - j, np.abs(i - j), 128 + j - i)):
        valid = dist <= 128
        for h in range(16):
            E[:, typ, h, :] = np.where(valid, np.exp(-slopes[h] * dist), 0.0)
    return E.reshape(128, -1)


def _prep_shared(inp):
    w_in = np.asarray(inp["w_in"], np.float32)[0]
    q, k, v, cb, cc, cx, ga, gc = np.split(w_in, [1024, 1280, 1536, 2560, 3584, 4608, 5632], axis=1)
    wao = np.asarray(inp["w_attn_o"], np.float32)[0]
    wco = np.asarray(inp["w_conv_o"], np.float32)[0]
    wo = np.asarray(inp["w_out"], np.float32)[0]
    cols = []
    for c in range(8):
        cols += [q[:, c * 64:(c + 1) * 64], q[:, (8 + c) * 64:(9 + c) * 64]]
    for g in (0, 2, 1, 3):
        cols.append(k[:, g * 64:(g + 1) * 64])
    cols.append(v)
    for c in range(8):
        sl = slice(c * 128, (c + 1) * 128)
        cols += [cc[:, sl], cx[:, sl], cb[:, sl]]
    for c in range(8):
        sl = slice(c * 128, (c + 1) * 128)
        cols += [ga[:, sl], wao[:, sl]]
    for c in range(8):
        sl = slice(c * 128, (c + 1) * 128)
        cols += [gc[:, sl], wco[:, sl]]
    cols.append(wo)
    wbig = np.ascontiguousarray(np.concatenate(cols, axis=1))
    assert wbig.shape == (1024, WBIG_COLS)
    wr = np.asarray(inp["w_router"], np.float32)[0]
    wr_l = np.ascontiguousarray(wr.reshape(8, 128, 16).transpose(1, 0, 2).reshape(128, 128))
    cwv = np.asarray(inp["conv_w"], np.float32)[0]
    cw_l = np.ascontiguousarray(cwv.reshape(3, 8, 128).transpose(2, 1, 0).reshape(128, 24))
    gbs = [inp["ln0_g"], inp["ln0_b"], inp["ln1_g"][0], inp["ln1_b"][0], inp["ln2_g"][0], inp["ln2_b"][0]]
    gb = np.concatenate([np.broadcast_to(np.asarray(a, np.float32)[None, :], (128, D)) for a in gbs], axis=1)
    sinkb = np.broadcast_to(np.asarray(inp["attn_sink"], np.float32)[0][None, :], (128, 16))
    sh = {
        "wbig": wbig, "wr": wr_l, "cw": cw_l, "gb": np.ascontiguousarray(gb), "sinkb": np.ascontiguousarray(sinkb),
        "emat": _alibi_table(), "ident": np.eye(128, dtype=np.float32), "ones16": np.ones((16, 16), np.float32),
        "offs": (np.arange(64)[:, None] // 16 * SEQ).astype(np.float32),
        "w_gate": np.ascontiguousarray(np.asarray(inp["w_gate"], np.float32)[0]),
        "w_up": np.ascontiguousarray(np.asarray(inp["w_up"], np.float32)[0]),
        "w_down": np.ascontiguousarray(np.asarray(inp["w_down"], np.float32)[0]),
    }
    return sh


_NC_CACHE = {}


def kernel(**inputs):
    x = np.asarray(inputs["x"], np.float32)
    B = x.shape[0]
    nseq = B // NCORES
    sh = _prep_shared(inputs)
    if nseq not in _NC_CACHE:
        _NC_CACHE[nseq] = build(nseq)
    nc = _NC_CACHE[nseq]
    in_maps = []
    for c in range(NCORES):
        m = dict(sh)
        m["x"] = np.ascontiguousarray(x[c * nseq:(c + 1) * nseq].reshape(nseq * SEQ, D))
        in_maps.append(m)
    res = run_bass_kernel_spmd(nc, in_maps, core_ids=list(range(NCORES)))
    out = np.concatenate([r["out"].reshape(nseq, SEQ, D) for r in res.results], axis=0)
    return out.astype(np.float32)
```
